# Optimizing a Trainium2 kernel written in Bass

```python
import jax, jax.numpy as jnp
from jax import lax
import numpy as np

D_MODEL = 1024
BATCH = 8
SEQ = 8192
DEPTH = 2

RET_HEAD_DIM = 128
RET_WIDTH = D_MODEL // 2
RET_HEADS = RET_WIDTH // RET_HEAD_DIM
HGRN_HEAD_DIM = 128
HGRN_WIDTH = D_MODEL - RET_WIDTH
HGRN_HEADS = HGRN_WIDTH // HGRN_HEAD_DIM
MIX_WIDTH = RET_WIDTH + HGRN_WIDTH
IN_WIDTHS = [RET_WIDTH] * 4 + [HGRN_WIDTH] * 5
IN_COLS = sum(IN_WIDTHS)
RET_CHUNK = 128
HGRN_CHUNK = 64
ROPE_BASE = 10000.0
N_EXPERTS = 16
CAPACITY_FACTOR = 2
EXPERT_FF = 1024
NORM_EPS = 1e-6

kernel_name = "hymba_style_retention_hgrn2_expert_choice_encoder"


def rms_normalize(x):
    xf = x.astype(jnp.float32)
    return (xf * lax.rsqrt(jnp.mean(xf * xf, axis=-1, keepdims=True) + NORM_EPS)).astype(x.dtype)


def rms_norm(x, g):
    return rms_normalize(x) * g.astype(x.dtype)


def to_heads(t, n_heads):
    b, s, _ = t.shape
    return t.reshape(b, s, n_heads, -1).transpose(0, 2, 1, 3)


def from_heads(t):
    b, h, s, d = t.shape
    return t.transpose(0, 2, 1, 3).reshape(b, s, h * d)


def to_chunks(t, chunk):
    b, h, s, d = t.shape
    return t.reshape(b, h, s // chunk, chunk, d).transpose(2, 0, 1, 3, 4)


def from_chunks(t):
    n, b, h, c, d = t.shape
    return t.transpose(1, 2, 0, 3, 4).reshape(b, h, n * c, d)


def rotary(x, pos):
    half = x.shape[-1] // 2
    inv_freq = ROPE_BASE ** (-jnp.arange(half, dtype=jnp.float32) / half)
    ang = pos.astype(jnp.float32)[:, None] * inv_freq[None, :]
    cos = jnp.cos(ang).astype(x.dtype)
    sin = jnp.sin(ang).astype(x.dtype)
    x1, x2 = x[..., :half], x[..., half:]
    return jnp.concatenate([x1 * cos - x2 * sin, x1 * sin + x2 * cos], axis=-1)


def retention_scan(q, k, v, log_gamma, include_diag):
    b, h, s, dk = q.shape
    dv = v.shape[-1]
    dt = q.dtype
    L = RET_CHUNK
    t = jnp.arange(L, dtype=jnp.float32)
    diff = t[:, None] - t[None, :]
    mask = (diff >= 0) if include_diag else (diff > 0)
    lg = log_gamma[:, None, None]
    intra_decay = jnp.where(mask, jnp.exp(lg * jnp.maximum(diff, 0.0)), 0.0).astype(dt)
    q_decay = jnp.exp(log_gamma[:, None] * (t + 1.0))[:, :, None].astype(dt)
    k_decay = jnp.exp(log_gamma[:, None] * (L - 1.0 - t))[:, :, None].astype(dt)
    chunk_decay = jnp.exp(log_gamma * L)[:, None, None].astype(dt)

    def step(state, inp):
        qc, kc, vc = inp
        scores = jnp.einsum('bhid,bhjd->bhij', qc, kc) * intra_decay
        out = (jnp.einsum('bhij,bhjv->bhiv', scores, vc)
               + jnp.einsum('bhid,bhdv->bhiv', qc * q_decay, state))
        state = chunk_decay * state + jnp.einsum('bhjd,bhjv->bhdv', kc * k_decay, vc)
        return state, out

    state0 = jnp.zeros((b, h, dk, dv), dt)
    _, out = lax.scan(step, state0, (to_chunks(q, L), to_chunks(k, L), to_chunks(v, L)))
    return from_chunks(out)


def hgrn2_scan(q, k, v, log_f):
    b, h, s, dk = q.shape
    dv = v.shape[-1]
    dt = q.dtype
    L = HGRN_CHUNK
    causal = jnp.tril(jnp.ones((L, L), dtype=bool))[:, :, None]

    def step(state, inp):
        qc, kc, vc, gc = inp
        cum = jnp.cumsum(gc, axis=2)
        rel = jnp.exp(jnp.where(causal, cum[:, :, :, None, :] - cum[:, :, None, :, :], -jnp.inf)).astype(dt)
        scores = jnp.einsum('bhtd,bhsd,bhtsd->bhts', qc, kc, rel)
        out = (jnp.einsum('bhts,bhsv->bhtv', scores, vc)
               + jnp.einsum('bhtd,bhdv->bhtv', qc * jnp.exp(cum).astype(dt), state))
        cum_end = cum[:, :, -1, :]
        k_to_end = kc * jnp.exp(cum_end[:, :, None, :] - cum).astype(dt)
        state = (jnp.exp(cum_end).astype(dt)[..., None] * state
                 + jnp.einsum('bhsd,bhsv->bhdv', k_to_end, vc))
        return state, out

    state0 = jnp.zeros((b, h, dk, dv), dt)
    _, out = lax.scan(step, state0, (to_chunks(q, L), to_chunks(k, L), to_chunks(v, L), to_chunks(log_f, L)))
    return from_chunks(out)


def flip_seq(t):
    return jnp.flip(t, axis=2)


def hybrid_mixer(xn, w_in, ret_g, hgrn_g, w_out, lb):
    b, s, _ = xn.shape
    dt = xn.dtype
    proj = xn @ w_in
    splits = np.cumsum(IN_WIDTHS)[:-1].tolist()
    rq, rk, rv, rg, hq, hf_fwd, hf_bwd, hi, hg = jnp.split(proj, splits, axis=-1)

    pos = jnp.arange(s)
    q = rotary(to_heads(rq, RET_HEADS), pos)
    k = rotary(to_heads(rk, RET_HEADS), pos) * (RET_HEAD_DIM ** -0.5)
    v = to_heads(rv, RET_HEADS)
    log_gamma = jnp.log1p(-(2.0 ** (-5.0 - jnp.arange(RET_HEADS, dtype=jnp.float32))))
    r_out = (retention_scan(q, k, v, log_gamma, True)
             + flip_seq(retention_scan(flip_seq(q), flip_seq(k), flip_seq(v), log_gamma, False)))
    r_out = from_heads(rms_normalize(r_out)) * ret_g.astype(dt) * jax.nn.silu(rg)

    q2 = jax.nn.silu(to_heads(hq, HGRN_HEADS)) * (HGRN_HEAD_DIM ** -0.5)
    v2 = to_heads(hi, HGRN_HEADS)

    def forget(z):
        z = z.astype(jnp.float32)
        if lb is None:
            return jax.nn.log_sigmoid(z), jax.nn.sigmoid(-z).astype(dt)
        lbh = lb.astype(jnp.float32).reshape(HGRN_HEADS, 1, HGRN_HEAD_DIM)
        f = lbh + (1.0 - lbh) * jax.nn.sigmoid(z)
        return jnp.log(f), ((1.0 - lbh) * jax.nn.sigmoid(-z)).astype(dt)

    logf_f, k_f = forget(to_heads(hf_fwd, HGRN_HEADS))
    logf_b, k_b = forget(to_heads(hf_bwd, HGRN_HEADS))
    h_out = (hgrn2_scan(q2, k_f, v2, logf_f)
             + flip_seq(hgrn2_scan(flip_seq(q2), flip_seq(k_b), flip_seq(v2), flip_seq(logf_b))))
    h_out = rms_norm(from_heads(h_out), hgrn_g) * jax.nn.silu(hg)

    return jnp.concatenate([r_out, h_out], axis=-1) @ w_out


def expert_choice_ffn(xn, w_router, w_gate, w_up, w_down):
    b, s, d = xn.shape
    cap = CAPACITY_FACTOR * s // N_EXPERTS
    affinity = jax.nn.softmax((xn @ w_router).astype(jnp.float32), axis=-1)
    gate, idx = lax.top_k(jnp.swapaxes(affinity, 1, 2), cap)
    xe = jax.vmap(lambda xb, ib: xb[ib])(xn, idx)
    hid = (jax.nn.silu(jnp.einsum('becd,edf->becf', xe, w_gate))
           * jnp.einsum('becd,edf->becf', xe, w_up))
    ye = jnp.einsum('becf,efd->becd', hid, w_down) * gate[..., None].astype(xn.dtype)
    return jax.vmap(lambda yb, ib: jnp.zeros((s, d), yb.dtype).at[ib.reshape(-1)].add(yb.reshape(-1, d)))(ye, idx)


def setup_inputs(seed: int = 0) -> dict:
    key = jax.random.key(seed)
    ks = jax.random.split(key, 14)
    f32 = jnp.float32
    x = jax.random.normal(ks[0], (BATCH, SEQ, D_MODEL), f32)
    norm1_g = 1.0 + 0.02 * jax.random.normal(ks[1], (DEPTH, D_MODEL), f32)
    w_in = jax.random.normal(ks[2], (DEPTH, D_MODEL, IN_COLS), f32) * D_MODEL ** -0.5
    ret_norm_g = 1.0 + 0.02 * jax.random.normal(ks[3], (DEPTH, RET_WIDTH), f32)
    hgrn_norm_g = 1.0 + 0.02 * jax.random.normal(ks[4], (DEPTH, HGRN_WIDTH), f32)
    w_out = jax.random.normal(ks[5], (DEPTH, MIX_WIDTH, D_MODEL), f32) * MIX_WIDTH ** -0.5
    lower_bounds = 0.1 * jax.random.normal(ks[6], (DEPTH, HGRN_WIDTH), f32)
    norm2_g = 1.0 + 0.02 * jax.random.normal(ks[7], (DEPTH, D_MODEL), f32)
    w_router = jax.random.normal(ks[8], (DEPTH, D_MODEL, N_EXPERTS), f32) * D_MODEL ** -0.5
    w_gate = jax.random.normal(ks[9], (DEPTH, N_EXPERTS, D_MODEL, EXPERT_FF), f32) * D_MODEL ** -0.5
    w_up = jax.random.normal(ks[10], (DEPTH, N_EXPERTS, D_MODEL, EXPERT_FF), f32) * D_MODEL ** -0.5
    w_down = jax.random.normal(ks[11], (DEPTH, N_EXPERTS, EXPERT_FF, D_MODEL), f32) * EXPERT_FF ** -0.5
    final_norm_g = 1.0 + 0.02 * jax.random.normal(ks[12], (D_MODEL,), f32)
    return {"x": x, "norm1_g": norm1_g, "w_in": w_in, "ret_norm_g": ret_norm_g,
            "hgrn_norm_g": hgrn_norm_g, "w_out": w_out, "lower_bounds": lower_bounds,
            "norm2_g": norm2_g, "w_router": w_router, "w_gate": w_gate, "w_up": w_up,
            "w_down": w_down, "final_norm_g": final_norm_g}


def reference(x, norm1_g, w_in, ret_norm_g, hgrn_norm_g, w_out, lower_bounds,
              norm2_g, w_router, w_gate, w_up, w_down, final_norm_g):
    lbs = jax.nn.softmax(lower_bounds.astype(jnp.float32), axis=0)
    lbs = jnp.cumsum(lbs, axis=0) - lbs[0]
    h = x
    for layer in range(DEPTH):
        lb = lbs[layer] if layer > 0 else None
        h = h + hybrid_mixer(rms_norm(h, norm1_g[layer]), w_in[layer], ret_norm_g[layer],
                             hgrn_norm_g[layer], w_out[layer], lb)
        h = h + expert_choice_ffn(rms_norm(h, norm2_g[layer]), w_router[layer],
                                  w_gate[layer], w_up[layer], w_down[layer])
    return rms_norm(h, final_norm_g)
```

```python
import os
import numpy as np
from contextlib import ExitStack
import concourse.bass as bass
import concourse.mybir as mybir
from concourse.bass_utils import run_bass_kernel_spmd

F32 = mybir.dt.float32
BF16 = mybir.dt.bfloat16
I32 = mybir.dt.int32
U32 = mybir.dt.uint32
AF = mybir.ActivationFunctionType
ALU = mybir.AluOpType
AX = mybir.AxisListType

D = 1024
NE = 16
EPS = 1e-6
INC = 4608


class Res:
    __slots__ = ("name", "last_w", "readers")

    def __init__(self, name=""):
        self.name = name
        self.last_w = None
        self.readers = {}


class T:
    def __init__(self, h, name=""):
        self.h = h
        self.res = Res(name)

    def __getitem__(self, k):
        return self.h[k]


class Prog:
    COMPUTE = ("pe", "act", "dve", "pool")
    QUEUES = ("pe", "act", "dve", "pool", "sp")

    def __init__(self, nc, same_engine_sync=True):
        self.nc = nc
        self.es = ExitStack()
        self.stacks = []
        self.ops = {e: [] for e in self.QUEUES}
        self.cnt = {}
        self.sems = {}
        self.waited = {e: {} for e in self.QUEUES}
        self.same_engine_sync = same_engine_sync
        self.free_dsems = {"hw": [], "sw": []}
        self.scope_dsems = []
        self.sem_kind = {}
        self.n_dsems = 0
        for e in self.COMPUTE:
            self._mksem(e)
        self._uid = 0

    def _mksem(self, key):
        self.sems[key] = self.es.enter_context(self.nc.semaphore("s_" + str(key)))
        self.cnt[key] = 0

    def dsem(self, kind="hw"):
        if self.free_dsems[kind]:
            k = self.free_dsems[kind].pop()
        else:
            self.n_dsems += 1
            k = "d%d" % self.n_dsems
            self._mksem(k)
            self.sem_kind[k] = kind
        if self.scope_dsems:
            self.scope_dsems[-1].append(k)
        return k

    def dsems(self, n, kind="hw"):
        return [self.dsem(kind) for _ in range(n)]

    def _stack(self):
        return self.stacks[-1] if self.stacks else self.es

    def push(self):
        self.stacks.append(ExitStack())
        self.scope_dsems.append([])

    def pop(self):
        self.barrier()
        self.stacks.pop().close()
        for k in self.scope_dsems.pop():
            self.free_dsems[self.sem_kind[k]].append(k)

    def sb(self, shape, dtype, name=None):
        self._uid += 1
        name = (name or "sb") + "_%d" % self._uid
        t = self._stack().enter_context(self.nc.sbuf_tensor(name, list(shape), dtype))
        return T(t, name)

    def ps(self, shape, dtype=F32, name=None):
        self._uid += 1
        name = (name or "ps") + "_%d" % self._uid
        t = self._stack().enter_context(self.nc.psum_tensor(name, list(shape), dtype))
        return T(t, name)

    def dram(self, name, shape, dtype, kind="Internal"):
        t = self.nc.dram_tensor(name, list(shape), dtype, kind=kind)
        return T(t.ap(), name)

    @staticmethod
    def _res(xs):
        out = []
        for x in xs or ():
            if x is None:
                continue
            if isinstance(x, (list, tuple)):
                out.extend(Prog._res(x))
            else:
                out.append(x.res if isinstance(x, T) else x)
        return out

    def op(self, eng, fns, reads=(), writes=(), dsem=None):
        if callable(fns):
            fns = [fns]
        reads = self._res(reads)
        writes = self._res(writes)
        deps = {}

        def add(tok):
            if tok is None:
                return
            k, v = tok
            if deps.get(k, 0) < v:
                deps[k] = v

        for r in reads:
            add(r.last_w)
        for w in writes:
            add(w.last_w)
            for k, v in w.readers.items():
                add((k, v))
        if dsem is not None:
            assert self.sem_kind[dsem] == ("sw" if eng == "pool" else "hw"), (eng, dsem)
            if self.cnt[dsem] > 0:
                add((dsem, self.cnt[dsem]))
            self.cnt[dsem] += 16
            tok = (dsem, self.cnt[dsem])
            inc = (dsem, 16)
        else:
            self.cnt[eng] += 1
            tok = (eng, self.cnt[eng])
            inc = (eng, 1)
        for w in writes:
            w.last_w = tok
            w.readers = {}
        for r in reads:
            if r.readers.get(tok[0], 0) < tok[1]:
                r.readers[tok[0]] = tok[1]
        waits = []
        wd = self.waited[eng]
        for k, v in deps.items():
            if k == eng and dsem is None:
                if eng == "pe" or not self.same_engine_sync:
                    continue
            if wd.get(k, 0) >= v:
                continue
            wd[k] = v
            waits.append((k, v))
        self.ops[eng].append((waits, fns, inc))
        return tok

    def barrier(self):
        snap = dict(self.cnt)
        for eng in self.QUEUES:
            waits = []
            wd = self.waited[eng]
            for k, v in snap.items():
                if v > 0 and wd.get(k, 0) < v:
                    wd[k] = v
                    waits.append((k, v))
            if waits:
                self.ops[eng].append((waits, [], None))

    def dma(self, q, out, in_, dsem, reads=(), writes=(), **kw):
        return self.op(q, lambda e: e.dma_start(out=out, in_=in_, **kw), reads, writes, dsem=dsem)

    def emit(self):
        nc = self.nc
        self.barrier()
        engmap = {"pe": "tensor", "act": "scalar", "dve": "vector", "pool": "gpsimd", "sp": "sync"}
        with nc.Block() as block:
            for ename, attr in engmap.items():
                ops = self.ops[ename]

                def body(e, ops=ops):
                    for waits, fns, inc in ops:
                        for k, v in waits:
                            e.wait_ge(self.sems[k], v)
                        ins = None
                        for f in fns:
                            ins = f(e)
                        if inc is not None:
                            ins.then_inc(self.sems[inc[0]], inc[1])

                getattr(block, attr)(body)

    def close(self):
        while self.stacks:
            self.stacks.pop().close()
        self.es.close()

    def tt(self, eng, out, in0, in1, op, reads, writes):
        return self.op(eng, lambda e: e.tensor_tensor(out=out, in0=in0, in1=in1, op=op), reads, writes)

    def ts(self, eng, out, in0, s1, s2, op0, op1, reads, writes):
        if op1 is None:
            return self.op(eng, lambda e: e.tensor_scalar(out=out, in0=in0, scalar1=s1, scalar2=None, op0=op0),
                           reads, writes)
        return self.op(eng, lambda e: e.tensor_scalar(out=out, in0=in0, scalar1=s1, scalar2=s2, op0=op0, op1=op1),
                       reads, writes)

    def stt(self, out, in0, scalar, in1, op0, op1, reads, writes):
        return self.op("dve", lambda e: e.scalar_tensor_tensor(out=out, in0=in0, scalar=scalar, in1=in1,
                                                               op0=op0, op1=op1), reads, writes)

    def act(self, out, in_, func, reads, writes, **kw):
        return self.op("act", lambda e: e.activation(out=out, in_=in_, func=func, **kw), reads, writes)

    def cp(self, eng, out, in_, reads, writes):
        if eng == "act":
            return self.op("act", lambda e: e.copy(out=out, in_=in_), reads, writes)
        return self.op(eng, lambda e: e.tensor_copy(out=out, in_=in_), reads, writes)


class Rec:
    def __init__(self):
        self.calls = []

    def __getattr__(self, name):
        def f(*a, **kw):
            self.calls.append((name, a, kw))
        return f


def interleave(p, lists):
    def run(c):
        getattr(p, c[0])(*c[1], **c[2])
    lists = [(x if isinstance(x, tuple) else (x, len(x) // 2)) for x in lists]
    if True:
        for x, _ in lists:
            for c in x:
                run(c)
        return
    n = len(lists)
    if n == 0:
        return
    for c in lists[0][0][:lists[0][1]]:
        run(c)
    for i in range(n):
        a = lists[i][0][lists[i][1]:]
        b = lists[i + 1][0][:lists[i + 1][1]] if i + 1 < n else []
        for j in range(max(len(a), len(b))):
            if j < len(a):
                run(a[j])
            if j < len(b):
                run(b[j])


def make_consts(S):
    c = {}
    c["c_ident"] = np.eye(128, dtype=np.float32)
    half = 64
    inv_freq = (10000.0 ** (-np.arange(half, dtype=np.float32) / np.float32(half))).astype(np.float32)
    ang = (np.arange(S, dtype=np.float32)[:, None] * inv_freq[None, :]).astype(np.float32)
    cos = np.cos(ang).astype(np.float32)
    sin = np.sin(ang).astype(np.float32)
    c["c_rope"] = np.concatenate([cos, cos, -sin, sin], axis=1).astype(np.float32)
    p = np.arange(128)
    same = (p[:, None] // 64) == (p[None, :] // 64)
    tri = np.zeros((128, 4, 128), np.float32)
    tri[:, 0, :] = same & (p[:, None] <= p[None, :])
    tri[:, 1, :] = same & (p[:, None] >= p[None, :])
    tri[:, 2, :] = same & (p[:, None] > p[None, :])
    tri[:, 3, :] = same & (p[:, None] < p[None, :])
    c["c_tri"] = tri
    j = np.arange(64)
    m = np.zeros((64, 2, 8, 64), np.float32)
    m[:, 0, :, :] = (j[:, None] <= j[None, :])[:, None, :]
    m[:, 1, 0:4, :] = (j[:, None] > j[None, :])[:, None, :]
    m[:, 1, 4:8, :] = (j[:, None] >= j[None, :])[:, None, :]
    c["c_mask"] = m
    lg = np.log1p(-(2.0 ** (-5.0 - np.arange(4, dtype=np.float64))))
    jj = (p % 64).astype(np.float64)[:, None]
    sc = 128.0 ** -0.5
    rd = np.zeros((128, 6, 4), np.float64)
    rd[:, 0] = np.exp((jj + 1) * lg)
    rd[:, 1] = np.exp(-(jj + 1) * lg) * sc
    rd[:, 2] = np.exp((63 - jj) * lg) * sc
    rd[:, 3] = np.exp((64 - jj) * lg)
    rd[:, 4] = np.exp(-(64 - jj) * lg) * sc
    rd[:, 5] = np.exp(jj * lg) * sc
    c["c_rdec"] = rd.astype(np.float32)
    rt = np.stack([rd[:, 0], rd[:, 1], rd[:, 3], rd[:, 4]], 0)
    c["c_rtab"] = np.ascontiguousarray(rt.transpose(0, 2, 1)).reshape(1, 4 * 4 * 128).astype(np.float32)
    c["c_rcd"] = np.broadcast_to(np.exp(64 * lg)[None, :], (128, 4)).astype(np.float32).copy()
    tk_ = np.zeros((128, S // 128, 2), np.float32)
    tk_[:, :, 0] = np.arange(S // 128)[None, :]
    tk_[:, :, 1] = np.arange(128)[:, None]
    c["c_tok"] = tk_
    c["c_tstrict"] = (p[:, None] < p[None, :]).astype(np.float32)
    return c


CONST_SHAPES = {
    "c_ident": lambda S: [128, 128], "c_rope": lambda S: [S, 256], "c_tri": lambda S: [128, 4, 128], "c_rtab": lambda S: [1, 2048],
    "c_mask": lambda S: [64, 2, 8, 64], "c_rdec": lambda S: [128, 6, 4], "c_rcd": lambda S: [128, 4],
    "c_tstrict": lambda S: [128, 128], "c_tok": lambda S: [128, S // 128, 2],
}


import os
SKIP = set()


def build(S, L, dbg=False, stop_after=None):
    stop_layer = int(os.environ.get("STOPL", "0"))
    stop_req = stop_after
    NT = S // 128
    NC = S // 64
    CAP = 2 * S // NE
    KT = CAP // 128
    NS = min(512, CAP)
    NSH = CAP // NS
    nc = bass.Bass("TRN2", target_bir_lowering=False)
    p = Prog(nc, same_engine_sync=True)
    skind = "ExternalOutput" if dbg else "Internal"

    x_in = p.dram("x", [S, D], F32, kind="ExternalInput")
    W = {}
    for nm, shp in [("norm1_g", [L, D]), ("w_in", [L, D, INC]), ("ret_norm_g", [L, 512]),
                    ("hgrn_norm_g", [L, 512]), ("w_out", [L, D, D]), ("lower_bounds", [L, 512]),
                    ("norm2_g", [L, D]), ("w_router", [L, D, NE]), ("w_gate", [L, NE, D, D]),
                    ("w_up", [L, NE, D, D]), ("w_down", [L, NE, D, D]), ("final_norm_g", [1, D])]:
        W[nm] = p.dram(nm, shp, F32, kind="ExternalInput")
    C = {k: p.dram(k, f(S), F32, kind="ExternalInput") for k, f in CONST_SHAPES.items()}
    out_d = p.dram("out", [S, D], F32, kind="ExternalOutput")

    qkT_d = [p.dram("s_qkT%d" % d, [NT, 128, 2, 8, 128], BF16, kind=skind) for d in range(2)]
    khat_d = [p.dram("s_khat%d" % d, [S, D], BF16, kind=skind) for d in range(2)]
    vv_d = p.dram("s_vv", [S, D], BF16, kind=skind)
    gates_d = p.dram("s_gates", [S, D], BF16, kind=skind)
    o_d = [p.dram("s_o%d" % d, [S, D], F32, kind=skind) for d in range(2)]
    RW = 1088
    hbuf = [p.dram("s_h%d" % i, [S, D], F32, kind=skind) for i in range(2)]
    xn2_d = p.dram("s_xn2", [S, D], BF16, kind=skind)
    xe_d = [p.dram("s_xe%d" % e, [CAP, RW], BF16, kind=skind) for e in range(NE)]
    dbg_d = {}
    if dbg:
        dbg_d["aff"] = p.dram("s_aff", [128, NE, NT], F32, kind=skind)
        dbg_d["pos"] = p.dram("s_pos", [128, NE, NT], I32, kind=skind)
        dbg_d["thr"] = p.dram("s_thr", [128, NE], F32, kind=skind)
        dbg_d["dec0"] = p.dram("s_dec0", [128, NC, 8], F32, kind=skind)
        dbg_d["dec1"] = p.dram("s_dec1", [128, NC, 8], F32, kind=skind)

    gsem = p.dsems(2, "sw")
    identb = p.sb([128, 128], BF16, "identb")
    p.dma("pool", identb[:], C["c_ident"][:], gsem[0], writes=[identb])
    ones = p.sb([128, 128], F32, "ones")
    p.op("dve", lambda e: e.memset(ones[:], 1.0), writes=[ones])
    onesb = p.sb([128, 128], BF16, "onesb")
    p.op("dve", lambda e: e.memset(onesb[:], 1.0), writes=[onesb])
    onec = p.sb([128, 1], F32, "onec")
    p.op("dve", lambda e: e.memset(onec[:], 1.0), writes=[onec])
    epsc = p.sb([128, 1], F32, "epsc")
    p.op("dve", lambda e: e.memset(epsc[:], EPS), writes=[epsc])

    def rms_rstd(ssum, n, out_rstd, width=1):
        p.act(out_rstd, ssum, AF.Ln, [ssum_res(ssum), epsc], [ssum_res(out_rstd)], scale=1.0 / n, bias=epsc[:])
        p.act(out_rstd, out_rstd, AF.Exp, [ssum_res(out_rstd)], [ssum_res(out_rstd)], scale=-0.5)

    _apres = {}

    def ssum_res(ap):
        return _apres[id(ap)]

    def reg(ap, t):
        _apres[id(ap)] = t
        return ap

    _regc = {}

    def sreg(e):
        if "s" not in _regc:
            _regc["s"] = e.to_reg(S - 1)
        return _regc["s"]

    def bcreg(e):
        if "bc" not in _regc:
            _regc["bc"] = e.to_reg(CAP - 1)
        return _regc["bc"]

    def layer(l, h_cur):
        stop_after = stop_req if l == stop_layer else None
        last = (l == L - 1)
        h1_d = hbuf[l % 2]
        h_next = h1_d
        p.push()
        dec = [p.sb([128, NC, 8], F32, "dec%d" % d) for d in range(2)]

        p.push()
        sA = p.dsems(16)
        sAw = p.dsems(3, "sw")
        win = p.sb([128, 8, INC], BF16, "win")
        win_k = [Res("win%d" % k) for k in range(8)]
        for k in range(8):
            p.dma("pool", win[:, k, :], W["w_in"][l, k * 128:(k + 1) * 128, :], sAw[k % 2], writes=[win_k[k]])
        g1b = p.sb([128, D], F32, "g1b")
        p.dma("sp", g1b[:], W["norm1_g"][l:l + 1, :].partition_broadcast(128), sA[2], writes=[g1b])
        tri = p.sb([128, 4, 128], BF16, "tri")
        p.dma("pool", tri[:], C["c_tri"][:], sAw[2], writes=[tri])
        rdec = p.sb([128, 6, 4], F32, "rdec")
        p.dma("sp", rdec[:], C["c_rdec"][:], sA[2], writes=[rdec])
        rcd = p.sb([128, 4], F32, "rcd")
        p.dma("sp", rcd[:], C["c_rcd"][:], sA[3], writes=[rcd])
        tabT = p.sb([128, 4, 4, 128], F32, "tabT")
        p.dma("sp", tabT[:].rearrange("p a h t -> p (a h t)"), C["c_rtab"][0:1, :].partition_broadcast(128), sA[2],
              writes=[tabT])
        for d in range(2):
            p.cp("dve", dec[d][:, :, 0:4], rcd[:].unsqueeze(1).to_broadcast([128, NC, 4]), [rcd], [dec[d]])
        use_lb = l > 0
        sgn = 1.0 if use_lb else -1.0
        if use_lb:
            assert l == 1
            lbv = p.sb([128, 512], F32, "lbv")
            oml = p.sb([128, 512], F32, "oml")
            p.dma("sp", lbv[:], W["lower_bounds"][1:2, :].partition_broadcast(128), sA[2], writes=[lbv])
            p.dma("sp", oml[:], W["lower_bounds"][0:1, :].partition_broadcast(128), sA[3], writes=[oml])
            p.tt("dve", oml[:], oml[:], lbv[:], ALU.subtract, [oml, lbv], [oml])
            p.act(oml[:], oml[:], AF.Exp, [oml], [oml])
            p.ts("dve", oml[:], oml[:], 1.0, None, ALU.add, None, [oml], [oml])
            p.op("dve", lambda e: e.reciprocal(out=lbv[:], in_=oml[:]), [oml], [lbv])
            p.ts("dve", oml[:], lbv[:], -1.0, 1.0, ALU.mult, ALU.add, [lbv], [oml])

        def dbl(shape, dt, nm):
            return [p.sb(shape, dt, "%s%d" % (nm, i)) for i in range(2)]

        xt = dbl([128, D], F32, "xt")
        rope = dbl([128, 256], F32, "rope")
        ss = dbl([128, 1], F32, "ss")
        rstd = dbl([128, 1], F32, "rstd")
        xn = dbl([128, D], BF16, "xn")
        xnT = dbl([128, 8, 128], BF16, "xnT")
        zlo = dbl([128, 8], F32, "zlo")
        q2, te, tf, tk, tcm, qa, qb, krot = [dbl([128, 512], F32, n) for n in
                                             ("q2", "te", "tf", "tk", "tcm", "qa", "qb", "krot")]
        tE1, tE2, tE3, thi, tlo = [dbl([128, 512], BF16, n) for n in ("tE1", "tE2", "tE3", "thi", "tlo")]
        tokQr = dbl([128, 512], BF16, "tokQr")
        tokKr = dbl([128, 512], BF16, "tokKr")
        hgQ = [dbl([128, 512], BF16, "hgQ%d" % d) for d in range(2)]
        hgK = [dbl([128, 512], BF16, "hgK%d" % d) for d in range(2)]
        khat = [dbl([128, D], BF16, "khat%d" % d) for d in range(2)]
        vtile = dbl([128, D], BF16, "vtile")
        gtile = dbl([128, D], BF16, "gtile")
        qkTs = [dbl([128, 2, 8, 128], BF16, "qkTs%d" % d) for d in range(2)]
        pT = p.ps([128, 8, 128], BF16, "pT")
        pG = [p.ps([128, 512], F32, "pG%d" % i) for i in range(2)]
        pC = p.ps([128, 2, 512], F32, "pC")
        pX = [p.ps([128, 8, 128], BF16, "pX%d" % i) for i in range(2)]
        pZ = p.ps([128, 512], F32, "pZ")
        rot = {"g": 0, "x": 0}

        def tile_ops(t):
            q = Rec()
            b = t % 2
            xb, rp = xt[b], rope[b]
            q.dma("sp", xb[:], h_cur[t * 128:(t + 1) * 128, :], sA[4 + b], writes=[xb])
            q.dma("sp", rp[:], C["c_rope"][t * 128:(t + 1) * 128, :], sA[6 + b], writes=[rp])
            q.act(xn[b][:], xb[:], AF.Square, [xb], [xn[b], ss[b]], accum_out=ss[b][:])
            q.act(rstd[b][:], ss[b][:], AF.Ln, [ss[b], epsc], [rstd[b]], scale=1.0 / D, bias=epsc[:])
            q.act(rstd[b][:], rstd[b][:], AF.Exp, [rstd[b]], [rstd[b]], scale=-0.5)
            q.stt(xn[b][:], xb[:], rstd[b][:], g1b[:], ALU.mult, ALU.mult, [xb, rstd[b], g1b], [xn[b]])
            q.op("pe", [lambda e, k=k: e.transpose(out=pT[:, k, :], in_=xn[b][:, k * 128:(k + 1) * 128],
                                                   identity=identb[:]) for k in range(8)], [xn[b], identb], [pT])
            q.cp("act", xnT[b][:], pT[:], [pT], [xnT[b]])

            def group(g):
                pg = pG[rot["g"] % 2]
                rot["g"] += 1
                q.op("pe", [lambda e, k=k: e.matmul(pg[:], lhsT=xnT[b][:, k, :], rhs=win[:, k, g * 512:(g + 1) * 512],
                                                    start=(k == 0), stop=(k == 7)) for k in range(8)],
                     [xnT[b]] + win_k, [pg])
                return pg

            def sig_chain(pg):
                q.act(te[b][:], pg[:], AF.Exp, [pg], [te[b]], scale=-1.0)
                q.act(te[b][:], te[b][:], AF.Ln, [te[b]], [te[b]], bias=onec[:])
                q.act(tf[b][:], te[b][:], AF.Exp, [te[b]], [tf[b]], scale=-1.0)

            def rotary(pg, dst_t, dst_ap):
                pg4 = pg[:].rearrange("p (h d) -> p h d", h=4)
                cs = rp[:, 0:128].unsqueeze(1).to_broadcast([128, 4, 128])
                sn1 = rp[:, 128:192].unsqueeze(1).to_broadcast([128, 4, 64])
                sn2 = rp[:, 192:256].unsqueeze(1).to_broadcast([128, 4, 64])
                qa4 = qa[b][:].rearrange("p (h d) -> p h d", h=4)
                qb4 = qb[b][:].rearrange("p (h d) -> p h d", h=4)
                q.tt("dve", qa4, pg4, cs, ALU.mult, [pg, rp], [qa[b]])
                q.op("dve", [lambda e: e.tensor_tensor(out=qb4[:, :, 0:64], in0=pg4[:, :, 64:128], in1=sn1, op=ALU.mult),
                             lambda e: e.tensor_tensor(out=qb4[:, :, 64:128], in0=pg4[:, :, 0:64], in1=sn2, op=ALU.mult)],
                     [pg, rp], [qb[b]])
                q.tt("dve", dst_ap, qa[b][:], qb[b][:], ALU.add, [qa[b], qb[b]], [dst_t])

            def forget(pg, d):
                sig_chain(pg)
                if use_lb:
                    lbs_ = ""
                    if "m" not in lbs_:
                        q.tt("dve", tf[b][:], tf[b][:], oml[:], ALU.mult, [tf[b], oml], [tf[b]])
                        q.tt("dve", tf[b][:], tf[b][:], lbv[:], ALU.add, [tf[b], lbv], [tf[b]])
                    if "c" in lbs_:
                        q.act(te[b][:], tf[b][:], AF.Identity, [tf[b]], [te[b]], scale=-0.5)
                    elif "l" not in lbs_:
                        q.act(te[b][:], tf[b][:], AF.Ln, [tf[b]], [te[b]])
                q.ts("dve", tk[b][:], tf[b][:], -1.0, 1.0, ALU.mult, ALU.add, [tf[b]], [tk[b]])
                q.cp("act", thi[b][:], te[b][:], [te[b]], [thi[b]])
                q.tt("dve", tlo[b][:], te[b][:], thi[b][:], ALU.subtract, [te[b], thi[b]], [tlo[b]])
                q.op("pe", [lambda e: e.matmul(pC[:, 0, :], lhsT=tri[:, d, :], rhs=thi[b][:], start=True, stop=False),
                            lambda e: e.matmul(pC[:, 0, :], lhsT=tri[:, d, :], rhs=tlo[b][:], start=False, stop=True),
                            lambda e: e.matmul(pC[:, 1, :], lhsT=tri[:, 2 + d, :], rhs=thi[b][:], start=True, stop=False),
                            lambda e: e.matmul(pC[:, 1, :], lhsT=tri[:, 2 + d, :], rhs=tlo[b][:], start=False, stop=True)],
                     [tri, thi[b], tlo[b]], [pC])
                fns = []
                for h in range(4):
                    for c2 in range(2):
                        for part, src in enumerate((thi[b], tlo[b])):
                            col = part * 8 + h * 2 + c2
                            fns.append(lambda e, h=h, c2=c2, col=col, src=src: e.matmul(
                                pZ[:, col:col + 1], lhsT=src[c2 * 64:(c2 + 1) * 64, h * 128:(h + 1) * 128],
                                rhs=onesb[c2 * 64:(c2 + 1) * 64, 0:1], start=True, stop=True))
                q.op("pe", fns, [thi[b], tlo[b], onesb], [pZ])
                q.cp("act", zlo[b][:], pZ[:, 8:16], [pZ], [zlo[b]])
                q.tt("dve", zlo[b][:], pZ[:, 0:8], zlo[b][:], ALU.add, [pZ, zlo[b]], [zlo[b]])
                q.act(dec[d][:, 2 * t:2 * t + 2, 4:8], zlo[b][:].rearrange("p (h c) -> p c h", c=2), AF.Exp,
                      [zlo[b]], [dec[d]], scale=sgn)
                if use_lb:
                    q.ts("dve", tcm[b][:], pC[:, 0, :], -80.0, None, ALU.max, None, [pC], [tcm[b]])
                else:
                    q.ts("dve", tcm[b][:], pC[:, 0, :], 80.0, None, ALU.min, None, [pC], [tcm[b]])
                q.act(tE1[b][:], tcm[b][:], AF.Exp, [tcm[b]], [tE1[b]], scale=sgn)
                q.act(tE2[b][:], tcm[b][:], AF.Exp, [tcm[b]], [tE2[b]], scale=-sgn)
                q.act(tE3[b][:], pC[:, 1, :], AF.Exp, [pC], [tE3[b]], scale=sgn)
                q.stt(hgQ[d][b][:], q2[b][:], 128.0 ** -0.5, tE1[b][:], ALU.mult, ALU.mult, [q2[b], tE1[b]], [hgQ[d][b]])
                q.tt("dve", hgK[d][b][:], tk[b][:], tE2[b][:], ALU.mult, [tk[b], tE2[b]], [hgK[d][b]])
                q.tt("dve", khat[d][b][:, 512:1024], tk[b][:], tE3[b][:], ALU.mult, [tk[b], tE3[b]], [khat[d][b]])

            pg = group(4)
            sig_chain(pg)
            q.tt("dve", q2[b][:], pg[:], tf[b][:], ALU.mult, [pg, tf[b]], [q2[b]])
            pg = group(5)
            forget(pg, 0)
            pg = group(6)
            forget(pg, 1)
            split = len(q.calls)
            pg = group(0)
            rotary(pg, tokQr[b], tokQr[b][:])
            pg = group(1)
            rotary(pg, krot[b], krot[b][:])
            q.cp("act", tokKr[b][:], krot[b][:], [krot[b]], [tokKr[b]])
            for d, ti in ((0, 2), (1, 5)):
                tb = rdec[:, ti, :].unsqueeze(2).to_broadcast([128, 4, 128])
                q.tt("dve", khat[d][b][:, 0:512].rearrange("p (h d) -> p h d", h=4),
                     krot[b][:].rearrange("p (h d) -> p h d", h=4), tb, ALU.mult, [krot[b], rdec], [khat[d][b]])
            pg = group(3)
            sig_chain(pg)
            q.tt("dve", gtile[b][:, 0:512], pg[:], tf[b][:], ALU.mult, [pg, tf[b]], [gtile[b]])
            pg = group(8)
            sig_chain(pg)
            q.tt("dve", gtile[b][:, 512:1024], pg[:], tf[b][:], ALU.mult, [pg, tf[b]], [gtile[b]])
            pg = group(2)
            q.cp("act", vtile[b][:, 0:512], pg[:], [pg], [vtile[b]])
            pg = group(7)
            q.cp("act", vtile[b][:, 512:1024], pg[:], [pg], [vtile[b]])
            for d in range(2):
                for wi in range(2):
                    px = pX[rot["x"] % 2]
                    rot["x"] += 1
                    sr = tokQr[b] if wi == 0 else tokKr[b]
                    sh = hgQ[d][b] if wi == 0 else hgK[d][b]
                    fns = [lambda e, h=h, sr=sr, px=px: e.transpose(out=px[:, h, :], in_=sr[:, h * 128:(h + 1) * 128],
                                                                   identity=identb[:]) for h in range(4)]
                    fns += [lambda e, h=h, sh=sh, px=px: e.transpose(out=px[:, 4 + h, :], in_=sh[:, h * 128:(h + 1) * 128],
                                                                   identity=identb[:]) for h in range(4)]
                    q.op("pe", fns, [sr, sh, identb], [px])
                    q.tt("dve", qkTs[d][b][:, wi, 0:4, :], px[:, 0:4, :], tabT[:, 2 * d + wi, :, :], ALU.mult,
                         [px, tabT], [qkTs[d][b]])
                    q.cp("act", qkTs[d][b][:, wi, 4:8, :], px[:, 4:8, :], [px], [qkTs[d][b]])
                q.dma("sp", qkT_d[d][t], qkTs[d][b][:], sA[8 + d], reads=[qkTs[d][b]], writes=[qkT_d[d]])
                q.dma("sp", khat_d[d][t * 128:(t + 1) * 128, :], khat[d][b][:], sA[10 + d], reads=[khat[d][b]],
                      writes=[khat_d[d]])
            q.dma("sp", vv_d[t * 128:(t + 1) * 128, :], vtile[b][:], sA[12], reads=[vtile[b]], writes=[vv_d])
            q.dma("sp", gates_d[t * 128:(t + 1) * 128, :], gtile[b][:], sA[13], reads=[gtile[b]], writes=[gates_d])
            return q.calls, split

        interleave(p, [tile_ops(t) for t in range(NT)])
        if dbg:
            for d in range(2):
                p.dma("sp", dbg_d["dec%d" % d][:], dec[d][:], sA[14 + d], reads=[dec[d]], writes=[dbg_d["dec%d" % d]])
        p.pop()
        if stop_after == "A":
            return None

        p.push()
        sF = p.dsems(16)
        mask = p.sb([64, 2, 8, 64], F32, "mask")
        p.dma("sp", mask[:], C["c_mask"][:], sF[0], writes=[mask])
        Sst = [p.sb([128, 8, 128], F32, "Sst%d" % d) for d in range(2)]
        Sbf = [p.sb([128, 8, 128], BF16, "Sbf%d" % d) for d in range(2)]
        for d in range(2):
            p.op("dve", lambda e, d=d: e.memset(Sst[d][:], 0.0), writes=[Sst[d]])
            p.op("pool", lambda e, d=d: e.memset(Sbf[d][:], 0.0), writes=[Sbf[d]])
        qkb = [[p.sb([128, 2, 8, 128], BF16, "qkb%d_%d" % (d, i)) for i in range(2)] for d in range(2)]
        khb = [[p.sb([64, D], BF16, "khb%d_%d" % (d, i)) for i in range(2)] for d in range(2)]
        vvb = [[p.sb([64, D], BF16, "vvb%d_%d" % (d, i)) for i in range(2)] for d in range(2)]
        smb = [p.sb([64, 8, 64], BF16, "smb%d" % d) for d in range(2)]
        osb = [[p.sb([64, D], F32, "osb%d_%d" % (d, i)) for i in range(2)] for d in range(2)]
        pS = [p.ps([64, 8, 64], F32, "pS%d" % d) for d in range(2)]
        pO = [p.ps([64, D], F32, "pO%d" % d) for d in range(2)]
        pD = p.ps([128, 8, 128], F32, "pD")

        def scan_loads(i, d):
            c = i if d == 0 else NC - 1 - i
            t, half = c // 2, c % 2
            first_half = (half == 0) if d == 0 else (half == 1)
            if first_half:
                qb_ = qkb[d][(i // 2) % 2]
                p.dma("sp", qb_[:], qkT_d[d][t], sF[1 + d * 2 + (i // 2) % 2], reads=[qkT_d[d]], writes=[qb_])
            kb_ = khb[d][i % 2]
            vb_ = vvb[d][i % 2]
            p.dma("sp", kb_[:], khat_d[d][c * 64:(c + 1) * 64, :], sF[5 + d * 2 + i % 2], reads=[khat_d[d]], writes=[kb_])
            p.dma("sp", vb_[:], vv_d[c * 64:(c + 1) * 64, :], sF[9 + d * 2 + i % 2], reads=[vv_d], writes=[vb_])

        for d in range(2):
            scan_loads(0, d)
        for i in range(NC):
            for d in range(2):
                if i + 1 < NC:
                    scan_loads(i + 1, d)
                c = i if d == 0 else NC - 1 - i
                half = c % 2
                qb_ = qkb[d][(i // 2) % 2]
                kb_ = khb[d][i % 2]
                vb_ = vvb[d][i % 2]
                cs = slice(half * 64, (half + 1) * 64)
                p.op("pe", [lambda e, h=h, d=d, qb_=qb_, cs=cs: e.matmul(
                    pS[d][:, h, :], lhsT=qb_[:, 1, h, cs], rhs=qb_[:, 0, h, cs], start=True, stop=True)
                            for h in range(8)], [qb_], [pS[d]])
                p.tt("dve", smb[d][:], pS[d][:], mask[:, d, :, :], ALU.mult, [pS[d], mask], [smb[d]])
                fns = []
                for h in range(8):
                    hs = slice(h * 128, (h + 1) * 128)
                    fns.append(lambda e, h=h, hs=hs, d=d, vb_=vb_: e.matmul(
                        pO[d][:, hs], lhsT=smb[d][:, h, :], rhs=vb_[:, hs], start=True, stop=False))
                    fns.append(lambda e, h=h, hs=hs, d=d, qb_=qb_, cs=cs: e.matmul(
                        pO[d][:, hs], lhsT=qb_[:, 0, h, cs], rhs=Sbf[d][:, h, :], start=False, stop=True))
                p.op("pe", fns, [smb[d], vb_, qb_, Sbf[d]], [pO[d]])
                ob_ = osb[d][i % 2]
                p.cp("act", ob_[:], pO[d][:], [pO[d]], [ob_])
                p.dma("sp", o_d[d][c * 64:(c + 1) * 64, :], ob_[:], sF[13 + d], reads=[ob_], writes=[o_d[d]])
                if i + 1 < NC:
                    p.op("pe", [lambda e, h=h, kb_=kb_, vb_=vb_: e.matmul(
                        pD[:, h, :], lhsT=kb_[:, h * 128:(h + 1) * 128], rhs=vb_[:, h * 128:(h + 1) * 128],
                        start=True, stop=True) for h in range(8)], [kb_, vb_], [pD])
                    p.op("dve", [lambda e, h=h, d=d, c=c: e.scalar_tensor_tensor(
                        out=Sst[d][:, h, :], in0=Sst[d][:, h, :], scalar=dec[d][:, c, h:h + 1], in1=pD[:, h, :],
                        op0=ALU.mult, op1=ALU.add) for h in range(8)], [Sst[d], dec[d], pD], [Sst[d]])
                    p.cp("act", Sbf[d][:], Sst[d][:], [Sst[d]], [Sbf[d]])
        p.pop()
        p.pop()
        if stop_after == "F":
            return None

        p.push()
        affT = p.sb([128, NE, NT], F32, "affT")
        gate_m = p.sb([128, NE, NT], F32, "gate_m")
        pos_i = p.sb([128, NE, NT], I32, "pos_i")

        p.push()
        sG = p.dsems(16)
        sGw = p.dsems(1, "sw")
        wout = p.sb([128, 8, D], BF16, "wout")
        p.dma("pool", wout[:], W["w_out"][l].rearrange("(k p) n -> p k n", p=128), sGw[0], writes=[wout])
        wrf = p.sb([128, 8, NE], F32, "wrf")
        p.dma("sp", wrf[:], W["w_router"][l].rearrange("(k p) n -> p k n", p=128), sG[1], writes=[wrf])
        wr = p.sb([128, 8, NE], BF16, "wr")
        wrl = p.sb([128, 8, NE], BF16, "wrl")
        p.cp("dve", wr[:], wrf[:], [wrf], [wr])
        p.tt("dve", wrl[:], wrf[:], wr[:], ALU.subtract, [wrf, wr], [wrl])
        gmix = p.sb([128, D], F32, "gmix")
        p.dma("sp", gmix[:, 0:512], W["ret_norm_g"][l:l + 1, :].partition_broadcast(128), sG[2], writes=[gmix])
        p.dma("sp", gmix[:, 512:1024], W["hgrn_norm_g"][l:l + 1, :].partition_broadcast(128), sG[2], writes=[gmix])
        g2b = p.sb([128, D], F32, "g2b")
        p.dma("sp", g2b[:], W["norm2_g"][l:l + 1, :].partition_broadcast(128), sG[3], writes=[g2b])
        ofb = [p.sb([128, D], F32, "ofb%d" % i) for i in range(2)]
        obb = [p.sb([128, D], F32, "obb%d" % i) for i in range(2)]
        gtb = [p.sb([128, D], BF16, "gtb%d" % i) for i in range(2)]
        hb = [p.sb([128, D], F32, "hb%d" % i) for i in range(2)]
        osum = p.sb([128, D], F32, "osum")
        gg = p.sb([128, D], F32, "gg")
        junkf = p.sb([128, D], BF16, "junkf")
        ss5 = p.sb([128, 8], F32, "ss5")
        rs5 = p.sb([128, 8], F32, "rs5")
        mixed = p.sb([128, D], BF16, "mixed")
        mT = p.sb([128, 8, 128], BF16, "mT")
        h1b = [p.sb([128, D], F32, "h1b%d" % i) for i in range(2)]
        xn2b = [p.sb([128, D], BF16, "xn2b%d" % i) for i in range(2)]
        x2T = p.sb([128, 8, 128], BF16, "x2T")
        x2Tl = p.sb([128, 8, 128], BF16, "x2Tl")
        xn2f = p.sb([128, D], F32, "xn2f")
        xn2l = p.sb([128, D], BF16, "xn2l")
        pT4 = p.ps([128, 8, 128], BF16, "pT4")
        ss2 = p.sb([128, 1], F32, "ss2")
        rs2 = p.sb([128, 1], F32, "rs2")
        mx = p.sb([128, 1], F32, "mx")
        sme = p.sb([128, 1], F32, "sme")
        ex = p.sb([128, NE], F32, "ex")
        pT2 = p.ps([128, 8, 128], BF16, "pT2")
        pT3 = p.ps([128, 8, 128], BF16, "pT3")
        pH = p.ps([128, D], F32, "pH")
        pL = p.ps([128, 512], F32, "pL")

        def g_loads(t):
            r = slice(t * 128, (t + 1) * 128)
            p.dma("sp", ofb[t % 2][:], o_d[0][r, :], sG[4 + t % 2], reads=[o_d[0]], writes=[ofb[t % 2]])
            p.dma("sp", obb[t % 2][:], o_d[1][r, :], sG[6 + t % 2], reads=[o_d[1]], writes=[obb[t % 2]])
            p.dma("sp", gtb[t % 2][:], gates_d[r, :], sG[8 + t % 2], reads=[gates_d], writes=[gtb[t % 2]])
            p.dma("sp", hb[t % 2][:], h_cur[r, :], sG[10 + t % 2], reads=[h_cur], writes=[hb[t % 2]])

        g_loads(0)
        for t in range(NT):
            if t + 1 < NT:
                g_loads(t + 1)
            r = slice(t * 128, (t + 1) * 128)
            of_, ob_, gt_, h_ = ofb[t % 2], obb[t % 2], gtb[t % 2], hb[t % 2]
            p.tt("dve", osum[:], of_[:], ob_[:], ALU.add, [of_, ob_], [osum])
            p.tt("dve", gg[:], gt_[:], gmix[:], ALU.mult, [gt_, gmix], [gg])
            fns = [lambda e, h=h: e.activation(out=junkf[:, h * 128:(h + 1) * 128], in_=osum[:, h * 128:(h + 1) * 128],
                                               func=AF.Square, accum_out=ss5[:, h:h + 1]) for h in range(4)]
            fns.append(lambda e: e.activation(out=junkf[:, 512:1024], in_=osum[:, 512:1024], func=AF.Square,
                                              accum_out=ss5[:, 4:5]))
            p.op("act", fns, [osum], [junkf, ss5])
            p.act(rs5[:, 0:4], ss5[:, 0:4], AF.Ln, [ss5, epsc], [rs5], scale=1.0 / 128, bias=epsc[:])
            p.act(rs5[:, 4:5], ss5[:, 4:5], AF.Ln, [ss5, epsc], [rs5], scale=1.0 / 512, bias=epsc[:])
            p.act(rs5[:, 0:5], rs5[:, 0:5], AF.Exp, [rs5], [rs5], scale=-0.5)
            fns = [lambda e, h=h: e.scalar_tensor_tensor(out=mixed[:, h * 128:(h + 1) * 128],
                                                         in0=osum[:, h * 128:(h + 1) * 128], scalar=rs5[:, h:h + 1],
                                                         in1=gg[:, h * 128:(h + 1) * 128], op0=ALU.mult, op1=ALU.mult)
                   for h in range(4)]
            fns.append(lambda e: e.scalar_tensor_tensor(out=mixed[:, 512:1024], in0=osum[:, 512:1024],
                                                        scalar=rs5[:, 4:5], in1=gg[:, 512:1024],
                                                        op0=ALU.mult, op1=ALU.mult))
            p.op("dve", fns, [osum, rs5, gg], [mixed])
            p.op("pe", [lambda e, k=k: e.transpose(out=pT2[:, k, :], in_=mixed[:, k * 128:(k + 1) * 128],
                                                   identity=identb[:]) for k in range(8)], [mixed, identb], [pT2])
            p.cp("act", mT[:], pT2[:], [pT2], [mT])
            fns = []
            for n2 in range(2):
                for k in range(8):
                    fns.append(lambda e, k=k, n2=n2: e.matmul(pH[:, n2 * 512:(n2 + 1) * 512], lhsT=mT[:, k, :],
                                                            rhs=wout[:, k, n2 * 512:(n2 + 1) * 512],
                                                            start=(k == 0), stop=(k == 7)))
            p.op("pe", fns, [mT, wout], [pH])
            h1_ = h1b[t % 2]
            p.tt("dve", h1_[:], pH[:], h_[:], ALU.add, [pH, h_], [h1_])
            p.dma("sp", h1_d[r, :], h1_[:], sG[12], reads=[h1_], writes=[h1_d])
            p.act(junkf[:], h1_[:], AF.Square, [h1_], [junkf, ss2], accum_out=ss2[:])
            p.act(rs2[:], ss2[:], AF.Ln, [ss2, epsc], [rs2], scale=1.0 / D, bias=epsc[:])
            p.act(rs2[:], rs2[:], AF.Exp, [rs2], [rs2], scale=-0.5)
            xn2_ = xn2b[t % 2]
            p.stt(xn2f[:], h1_[:], rs2[:], g2b[:], ALU.mult, ALU.mult, [h1_, rs2, g2b], [xn2f])
            p.cp("act", xn2_[:], xn2f[:], [xn2f], [xn2_])
            p.tt("dve", xn2l[:], xn2f[:], xn2_[:], ALU.subtract, [xn2f, xn2_], [xn2l])
            p.dma("sp", xn2_d[r, :], xn2_[:], sG[13], reads=[xn2_], writes=[xn2_d])
            p.op("pe", [lambda e, k=k, xn2_=xn2_: e.transpose(out=pT3[:, k, :], in_=xn2_[:, k * 128:(k + 1) * 128],
                                                            identity=identb[:]) for k in range(8)],
                 [xn2_, identb], [pT3])
            p.cp("dve", x2T[:], pT3[:], [pT3], [x2T])
            p.op("pe", [lambda e, k=k: e.transpose(out=pT4[:, k, :], in_=xn2l[:, k * 128:(k + 1) * 128],
                                                   identity=identb[:]) for k in range(8)],
                 [xn2l, identb], [pT4])
            p.cp("act", x2Tl[:], pT4[:], [pT4], [x2Tl])
            fns = []
            for part, (xa, wa) in enumerate(((x2T, wr), (x2Tl, wr), (x2T, wrl))):
                for k in range(8):
                    fns.append(lambda e, k=k, xa=xa, wa=wa, part=part: e.matmul(
                        pL[:, 0:NE], lhsT=xa[:, k, :], rhs=wa[:, k, :], start=(part == 0 and k == 0),
                        stop=(part == 2 and k == 7)))
            p.op("pe", fns, [x2T, x2Tl, wr, wrl], [pL])
            p.op("dve", lambda e: e.tensor_reduce(out=mx[:], in_=pL[:, 0:NE], axis=AX.X, op=ALU.max), [pL], [mx])
            p.ts("dve", mx[:], mx[:], -1.0, None, ALU.mult, None, [mx], [mx])
            p.act(ex[:], pL[:, 0:NE], AF.Exp, [pL, mx], [ex, sme], bias=mx[:], accum_out=sme[:])
            p.op("dve", lambda e: e.reciprocal(out=sme[:], in_=sme[:]), [sme], [sme])
            p.ts("dve", affT[:, :, t], ex[:], sme[:], None, ALU.mult, None, [ex, sme], [affT])
        p.pop()
        if stop_after == "G":
            return None

        p.push()
        sR = p.dsems(4)
        sRw = p.dsems(1, "sw")
        lo = p.sb([128, NE], F32, "lo")
        hi = p.sb([128, NE], F32, "hi")
        mid = p.sb([128, NE], F32, "mid")
        cmpb = p.sb([128, NE, NT], F32, "cmpb")
        cntp = p.sb([128, NE], F32, "cntp")
        cntpb = p.sb([128, NE], BF16, "cntpb")
        geu = p.sb([128, NE], U32, "geu")
        ltu = p.sb([128, NE], U32, "ltu")
        pcnt = p.ps([128, 512], F32, "pcnt")
        tstr = p.sb([128, 128], BF16, "tstr")
        p.dma("pool", tstr[:], C["c_tstrict"][:], sRw[0], writes=[tstr])
        p.op("dve", lambda e: e.memset(lo[:], 0.0), writes=[lo])
        p.op("dve", lambda e: e.memset(hi[:], 1.0), writes=[hi])
        for it in range(30):
            p.tt("dve", mid[:], lo[:], hi[:], ALU.add, [lo, hi], [mid])
            p.ts("dve", mid[:], mid[:], 0.5, None, ALU.mult, None, [mid], [mid])
            p.tt("dve", cmpb[:], affT[:], mid[:].unsqueeze(2).to_broadcast([128, NE, NT]), ALU.is_ge,
                 [affT, mid], [cmpb])
            p.op("dve", lambda e: e.tensor_reduce(out=cntp[:], in_=cmpb[:], axis=AX.X, op=ALU.add), [cmpb], [cntp])
            p.cp("dve", cntpb[:], cntp[:], [cntp], [cntpb])
            p.op("pe", lambda e: e.matmul(pcnt[:, 0:NE], lhsT=onesb[:], rhs=cntpb[:], start=True, stop=True),
                 [onesb, cntpb], [pcnt])
            p.ts("dve", geu[:], pcnt[:, 0:NE], float(CAP), None, ALU.is_ge, None, [pcnt], [geu])
            p.ts("dve", ltu[:], pcnt[:, 0:NE], float(CAP), None, ALU.is_lt, None, [pcnt], [ltu])
            p.op("dve", lambda e: e.copy_predicated(out=lo[:], mask=geu[:], data=mid[:]), [geu, mid], [lo])
            p.op("dve", lambda e: e.copy_predicated(out=hi[:], mask=ltu[:], data=mid[:]), [ltu, mid], [hi])
        msel = p.sb([128, NE, NT], F32, "msel")
        mselb = p.sb([128, NE, NT], BF16, "mselb")
        posf = p.sb([128, NE, NT], F32, "posf")
        tot = p.sb([128, NE, NT], F32, "tot")
        inc = p.sb([128, NE, NT], F32, "inc")
        p.tt("dve", msel[:], affT[:], lo[:].unsqueeze(2).to_broadcast([128, NE, NT]), ALU.is_ge, [affT, lo], [msel])
        p.cp("dve", mselb[:], msel[:], [msel], [mselb])
        NF = NE * NT
        pw = p.ps([128, max(NF, 512)], F32, "pw")
        pt_ = p.ps([128, max(NF, 512)], F32, "pt")
        mflat = mselb[:].rearrange("p e t -> p (e t)")
        fns = []
        for j in range(0, NF, 512):
            w = min(512, NF - j)
            fns.append(lambda e, j=j, w=w: e.matmul(pw[:, j:j + w], lhsT=tstr[:], rhs=mflat[:, j:j + w],
                                                    start=True, stop=True))
            fns.append(lambda e, j=j, w=w: e.matmul(pt_[:, j:j + w], lhsT=onesb[:], rhs=mflat[:, j:j + w],
                                                    start=True, stop=True))
        p.op("pe", fns, [tstr, onesb, mselb], [pw, pt_])
        p.cp("dve", tot[:].rearrange("p e t -> p (e t)"), pt_[:, 0:NF], [pt_], [tot])
        p.op("dve", [lambda e, ee=ee: e.tensor_tensor_scan(out=inc[:, ee, :], data0=ones[:, 0:NT], data1=tot[:, ee, :],
                                                           initial=0.0, op0=ALU.mult, op1=ALU.add)
                     for ee in range(NE)], [ones, tot], [inc])
        p.tt("dve", inc[:], inc[:], tot[:], ALU.subtract, [inc, tot], [inc])
        p.tt("dve", posf[:].rearrange("p e t -> p (e t)"), pw[:, 0:NF], inc[:].rearrange("p e t -> p (e t)"),
             ALU.add, [pw, inc], [posf])
        p.ts("dve", tot[:], posf[:], float(CAP), None, ALU.is_lt, None, [posf], [tot])
        p.tt("dve", msel[:], msel[:], tot[:], ALU.mult, [msel, tot], [msel])
        p.tt("dve", gate_m[:], affT[:], msel[:], ALU.mult, [affT, msel], [gate_m])
        p.ts("dve", tot[:], msel[:], -1.0e6, 1.0e6, ALU.mult, ALU.add, [msel], [tot])
        p.tt("dve", posf[:], posf[:], tot[:], ALU.add, [posf, tot], [posf])
        p.cp("dve", pos_i[:], posf[:], [posf], [pos_i])
        if dbg:
            p.dma("sp", dbg_d["aff"][:], affT[:], sR[1], reads=[affT], writes=[dbg_d["aff"]])
            p.dma("sp", dbg_d["pos"][:], pos_i[:], sR[2], reads=[pos_i], writes=[dbg_d["pos"]])
            p.dma("sp", dbg_d["thr"][:], lo[:], sR[3], reads=[lo], writes=[dbg_d["thr"]])
        p.pop()
        if stop_after == "R":
            return None

        p.push()
        sX = p.dsems(2) + p.dsems(8, "sw")
        xb2 = [p.sb([128, RW], BF16, "xb2_%d" % i) for i in range(2)]
        tok_i = p.sb([128, NT], I32, "tok_i")
        p.op("pool", lambda e: e.iota(tok_i[:], pattern=[[128, NT]], base=0, channel_multiplier=1), writes=[tok_i])
        for i in range(2):
            p.op("dve", lambda e, i=i: e.memset(xb2[i][:, 1024:RW], 0.0), writes=[xb2[i]])
        k = 0
        for t in range(NT):
            xb_ = xb2[t % 2]
            p.dma("sp", xb_[:, 0:1024], xn2_d[t * 128:(t + 1) * 128, :], sX[t % 2], reads=[xn2_d], writes=[xb_])
            p.cp("dve", xb_[:, 1024:1040], gate_m[:, :, t], [gate_m], [xb_])
            p.tt("dve", xb_[:, 1040:1056], gate_m[:, :, t], xb_[:, 1024:1040], ALU.subtract, [gate_m, xb_], [xb_])
            p.cp("dve", xb_[:, 1056:1058].bitcast(I32), tok_i[:, t:t + 1], [tok_i], [xb_])
            for ee in range(NE):
                p.op("pool", lambda e, ee=ee, t=t, xb_=xb_: e.indirect_dma_start(
                    out=xe_d[ee][:, :], out_offset=bass.IndirectOffsetOnAxis(ap=pos_i[:, ee, t:t + 1], axis=0),
                    in_=xb_[:], in_offset=None, bounds_check=bcreg(e), oob_is_err=False),
                     reads=[xb_, pos_i], writes=[], dsem=sX[2 + k % 8])
                k += 1
        p.pop()
        if stop_after == "X":
            return None

        p.push()
        sD = p.dsems(6, "sw") + p.dsems(2) + p.dsems(4, "sw")
        wgb = [p.sb([128, 8, D], BF16, "wg%d" % i) for i in range(2)]
        wub = [p.sb([128, 8, D], BF16, "wu%d" % i) for i in range(2)]
        wdb = [p.sb([128, 8, D], BF16, "wd%d" % i) for i in range(2)]
        xeb = [p.sb([128, KT, RW], BF16, "xeb%d" % i) for i in range(2)]
        xeT = p.sb([128, 8, CAP], BF16, "xeT")
        hidT = p.sb([128, 8, CAP], BF16, "hidT")
        sgb = [p.sb([128, NS], F32, "sgb%d" % i) for i in range(2)]
        yob = [p.sb([128, D], F32, "yob%d" % i) for i in range(3)]
        gsc = [p.sb([128, 1], F32, "gsc%d" % i) for i in range(3)]
        idxi = [p.sb([128, 1], I32, "idxi%d" % i) for i in range(3)]
        idxf = p.sb([128, 1], F32, "idxf")
        hres = Res("hacc")
        turn = p.sb([128, 8], F32, "turn")
        yi = 0
        pXe = p.ps([128, 8, 128], BF16, "pXe")
        pGt = [p.ps([128, 512], F32, "pGt%d" % i) for i in range(2)]
        pUp = [p.ps([128, 512], F32, "pUp%d" % i) for i in range(2)]
        pDn = p.ps([128, D], F32, "pDn")

        def d_loads(ee):
            b = ee % 2
            p.dma("pool", wgb[b][:], W["w_gate"][l, ee].rearrange("(k p) n -> p k n", p=128), sD[0 + b], writes=[wgb[b]])
            p.dma("pool", wub[b][:], W["w_up"][l, ee].rearrange("(k p) n -> p k n", p=128), sD[2 + b], writes=[wub[b]])
            p.dma("pool", wdb[b][:], W["w_down"][l, ee].rearrange("(k p) n -> p k n", p=128), sD[4 + b], writes=[wdb[b]])
            p.dma("sp", xeb[b][:], xe_d[ee][:, :].rearrange("(j p) d -> p j d", p=128), sD[6 + b], writes=[xeb[b]])

        d_loads(0)
        gi = 0
        for ee in range(NE):
            if ee + 1 < NE:
                d_loads(ee + 1)
            b = ee % 2
            wg_, wu_, wd_, xe_ = wgb[b], wub[b], wdb[b], xeb[b]
            for j in range(KT):
                p.op("pe", [lambda e, k=k, j=j, xe_=xe_: e.transpose(
                    out=pXe[:, k, :], in_=xe_[:, j, k * 128:(k + 1) * 128], identity=identb[:]) for k in range(8)],
                     [xe_, identb], [pXe])
                p.cp("act" if j % 2 == 0 else "dve", xeT[:, :, j * 128:(j + 1) * 128], pXe[:], [pXe], [xeT])
            for ft in range(8):
                fs = slice(ft * 128, (ft + 1) * 128)
                for sh in range(NSH):
                    ssl = slice(sh * NS, (sh + 1) * NS)
                    pg_, pu_ = pGt[gi % 2], pUp[gi % 2]
                    sg_ = sgb[gi % 2]
                    gi += 1
                    p.op("pe", [lambda e, k=k, fs=fs, ssl=ssl, pg_=pg_, wg_=wg_: e.matmul(
                        pg_[:, 0:NS], lhsT=wg_[:, k, fs], rhs=xeT[:, k, ssl], start=(k == 0), stop=(k == 7))
                                for k in range(8)], [wg_, xeT], [pg_])
                    p.op("pe", [lambda e, k=k, fs=fs, ssl=ssl, pu_=pu_, wu_=wu_: e.matmul(
                        pu_[:, 0:NS], lhsT=wu_[:, k, fs], rhs=xeT[:, k, ssl], start=(k == 0), stop=(k == 7))
                                for k in range(8)], [wu_, xeT], [pu_])
                    p.act(sg_[:], pg_[:, 0:NS], AF.Silu, [pg_], [sg_])
                    p.tt("dve", hidT[:, ft, ssl], sg_[:], pu_[:, 0:NS], ALU.mult, [sg_, pu_], [hidT])
            for j in range(KT):
                js = slice(j * 128, (j + 1) * 128)
                fns = []
                for n2 in range(2):
                    for ft in range(8):
                        fns.append(lambda e, ft=ft, n2=n2, js=js, wd_=wd_: e.matmul(
                            pDn[:, n2 * 512:(n2 + 1) * 512], lhsT=hidT[:, ft, js], rhs=wd_[:, ft, n2 * 512:(n2 + 1) * 512],
                            start=(ft == 0), stop=(ft == 7)))
                p.op("pe", fns, [hidT, wd_], [pDn])
                yo_ = yob[yi % 3]
                yi += 1
                gs_ = gsc[yi % 3]
                p.tt("dve", gs_[:], xe_[:, j, 1024 + ee:1025 + ee], xe_[:, j, 1040 + ee:1041 + ee], ALU.add, [xe_], [gs_])
                gate_ap = gs_[:]
                idx_ap = xe_[:, j, 1056:1058].bitcast(I32)
                p.act(yo_[:, 0:512], pDn[:, 0:512], AF.Identity, [pDn, gs_], [yo_], scale=gate_ap)
                p.ts("dve", yo_[:, 512:1024], pDn[:, 512:1024], gate_ap, None, ALU.mult, None, [pDn, gs_], [yo_])
                if j == 0:
                    p.op("pool", lambda e: e.memset(turn[:], 0.0), writes=[hres, turn])
                p.op("pool", lambda e, yo_=yo_, idx_ap=idx_ap: e.indirect_dma_start(
                    out=h1_d[:, :], out_offset=bass.IndirectOffsetOnAxis(ap=idx_ap, axis=0),
                    in_=yo_[:], in_offset=None, bounds_check=sreg(e), oob_is_err=True, compute_op=ALU.add),
                     reads=[yo_, xe_, hres], writes=[], dsem=sD[8 + (yi % 4)])
        p.pop()
        if stop_after == "D":
            return None

        p.pop()
        if last:
            p.push()
            sE = p.dsems(6)
            gfb = p.sb([128, D], F32, "gfb")
            p.dma("sp", gfb[:], W["final_norm_g"][0:1, :].partition_broadcast(128), sE[0], writes=[gfb])
            ssf = p.sb([128, 1], F32, "ssf")
            rsf = p.sb([128, 1], F32, "rsf")
            junke = p.sb([128, D], BF16, "junke")
            accb = [p.sb([128, D], F32, "acc%d" % i) for i in range(2)]
            outb = [p.sb([128, D], F32, "outb%d" % i) for i in range(2)]
            for t in range(NT):
                r = slice(t * 128, (t + 1) * 128)
                acc = accb[t % 2]
                p.dma("sp", acc[:], h1_d[r, :], sE[1 + t % 2], reads=[h1_d], writes=[acc])
                p.act(junke[:], acc[:], AF.Square, [acc], [junke, ssf], accum_out=ssf[:])
                p.act(rsf[:], ssf[:], AF.Ln, [ssf, epsc], [rsf], scale=1.0 / D, bias=epsc[:])
                p.act(rsf[:], rsf[:], AF.Exp, [rsf], [rsf], scale=-0.5)
                ob_ = outb[t % 2]
                p.stt(ob_[:], acc[:], rsf[:], gfb[:], ALU.mult, ALU.mult, [acc, rsf, gfb], [ob_])
                p.dma("sp", out_d[r, :], ob_[:], sE[3 + t % 2], reads=[ob_], writes=[out_d])
            p.pop()
        if stop_after == "E":
            return None
        return h_next


    h_cur = x_in
    for l in range(L):
        h_cur = layer(l, h_cur)
        if h_cur is None:
            break

    p.emit()
    p.close()
    return nc


_CACHE = {}


def kernel(x, norm1_g, w_in, ret_norm_g, hgrn_norm_g, w_out, lower_bounds, norm2_g, w_router,
           w_gate, w_up, w_down, final_norm_g):
    x = np.asarray(x, dtype=np.float32)
    B, S, _ = x.shape
    L = int(np.asarray(w_in).shape[0])
    key = (S, L)
    if key not in _CACHE:
        _CACHE[key] = build(S, L)
    nc = _CACHE[key]
    consts = make_consts(S)
    shared = {
        "norm1_g": norm1_g, "w_in": w_in, "ret_norm_g": ret_norm_g, "hgrn_norm_g": hgrn_norm_g,
        "w_out": w_out, "lower_bounds": lower_bounds, "norm2_g": norm2_g, "w_router": w_router,
        "w_gate": w_gate, "w_up": w_up, "w_down": w_down,
    }
    shared = {k: np.ascontiguousarray(np.asarray(v, dtype=np.float32)) for k, v in shared.items()}
    shared["final_norm_g"] = np.ascontiguousarray(np.asarray(final_norm_g, dtype=np.float32).reshape(1, D))
    shared.update(consts)
    in_maps = []
    for b in range(B):
        m = dict(shared)
        m["x"] = np.ascontiguousarray(x[b])
        in_maps.append(m)
    res = run_bass_kernel_spmd(nc, in_maps, core_ids=list(range(B)))
    return np.stack([np.asarray(r["out"], dtype=np.float32) for r in res.results], axis=0)
```

```python
import os
import numpy as np
from contextlib import ExitStack
import concourse.bass as bass
import concourse.mybir as mybir
from concourse.bass_utils import run_bass_kernel_spmd

F32 = mybir.dt.float32
BF16 = mybir.dt.bfloat16
I32 = mybir.dt.int32
U32 = mybir.dt.uint32
AF = mybir.ActivationFunctionType
ALU = mybir.AluOpType
AX = mybir.AxisListType

D = 1024
NE = 16
EPS = 1e-6
INC = 4608


class Res:
    __slots__ = ("name", "last_w", "readers")

    def __init__(self, name=""):
        self.name = name
        self.last_w = None
        self.readers = {}


class T:
    def __init__(self, h, name=""):
        self.h = h
        self.res = Res(name)

    def __getitem__(self, k):
        return self.h[k]


class Prog:
    COMPUTE = ("pe", "act", "dve", "pool")
    QUEUES = ("pe", "act", "dve", "pool", "sp")

    def __init__(self, nc, same_engine_sync=True):
        self.nc = nc
        self.es = ExitStack()
        self.stacks = []
        self.ops = {e: [] for e in self.QUEUES}
        self.cnt = {}
        self.sems = {}
        self.waited = {e: {} for e in self.QUEUES}
        self.same_engine_sync = same_engine_sync
        self.free_dsems = {"hw": [], "sw": []}
        self.scope_dsems = []
        self.sem_kind = {}
        self.n_dsems = 0
        for e in self.COMPUTE:
            self._mksem(e)
        self._uid = 0

    def _mksem(self, key):
        self.sems[key] = self.es.enter_context(self.nc.semaphore("s_" + str(key)))
        self.cnt[key] = 0

    def dsem(self, kind="hw"):
        if self.free_dsems[kind]:
            k = self.free_dsems[kind].pop()
        else:
            self.n_dsems += 1
            k = "d%d" % self.n_dsems
            self._mksem(k)
            self.sem_kind[k] = kind
        if self.scope_dsems:
            self.scope_dsems[-1].append(k)
        return k

    def dsems(self, n, kind="hw"):
        return [self.dsem(kind) for _ in range(n)]

    def _stack(self):
        return self.stacks[-1] if self.stacks else self.es

    def push(self):
        self.stacks.append(ExitStack())
        self.scope_dsems.append([])

    def pop(self):
        self.barrier()
        self.stacks.pop().close()
        for k in self.scope_dsems.pop():
            self.free_dsems[self.sem_kind[k]].append(k)

    def sb(self, shape, dtype, name=None):
        self._uid += 1
        name = (name or "sb") + "_%d" % self._uid
        t = self._stack().enter_context(self.nc.sbuf_tensor(name, list(shape), dtype))
        return T(t, name)

    def ps(self, shape, dtype=F32, name=None):
        self._uid += 1
        name = (name or "ps") + "_%d" % self._uid
        t = self._stack().enter_context(self.nc.psum_tensor(name, list(shape), dtype))
        return T(t, name)

    def dram(self, name, shape, dtype, kind="Internal"):
        t = self.nc.dram_tensor(name, list(shape), dtype, kind=kind)
        return T(t.ap(), name)

    @staticmethod
    def _res(xs):
        out = []
        for x in xs or ():
            if x is None:
                continue
            if isinstance(x, (list, tuple)):
                out.extend(Prog._res(x))
            else:
                out.append(x.res if isinstance(x, T) else x)
        return out

    def op(self, eng, fns, reads=(), writes=(), dsem=None):
        if callable(fns):
            fns = [fns]
        reads = self._res(reads)
        writes = self._res(writes)
        deps = {}

        def add(tok):
            if tok is None:
                return
            k, v = tok
            if deps.get(k, 0) < v:
                deps[k] = v

        for r in reads:
            add(r.last_w)
        for w in writes:
            add(w.last_w)
            for k, v in w.readers.items():
                add((k, v))
        if dsem is not None:
            assert self.sem_kind[dsem] == ("sw" if eng == "pool" else "hw"), (eng, dsem)
            if self.cnt[dsem] > 0:
                add((dsem, self.cnt[dsem]))
            self.cnt[dsem] += 16
            tok = (dsem, self.cnt[dsem])
            inc = (dsem, 16)
        else:
            self.cnt[eng] += 1
            tok = (eng, self.cnt[eng])
            inc = (eng, 1)
        for w in writes:
            w.last_w = tok
            w.readers = {}
        for r in reads:
            if r.readers.get(tok[0], 0) < tok[1]:
                r.readers[tok[0]] = tok[1]
        waits = []
        wd = self.waited[eng]
        for k, v in deps.items():
            if k == eng and dsem is None:
                if eng == "pe" or not self.same_engine_sync:
                    continue
            if wd.get(k, 0) >= v:
                continue
            wd[k] = v
            waits.append((k, v))
        self.ops[eng].append((waits, fns, inc))
        return tok

    def barrier(self):
        snap = dict(self.cnt)
        for eng in self.QUEUES:
            waits = []
            wd = self.waited[eng]
            for k, v in snap.items():
                if v > 0 and wd.get(k, 0) < v:
                    wd[k] = v
                    waits.append((k, v))
            if waits:
                self.ops[eng].append((waits, [], None))

    def dma(self, q, out, in_, dsem, reads=(), writes=(), **kw):
        return self.op(q, lambda e: e.dma_start(out=out, in_=in_, **kw), reads, writes, dsem=dsem)

    def emit(self):
        nc = self.nc
        self.barrier()
        engmap = {"pe": "tensor", "act": "scalar", "dve": "vector", "pool": "gpsimd", "sp": "sync"}
        with nc.Block() as block:
            for ename, attr in engmap.items():
                ops = self.ops[ename]

                def body(e, ops=ops):
                    for waits, fns, inc in ops:
                        for k, v in waits:
                            e.wait_ge(self.sems[k], v)
                        ins = None
                        for f in fns:
                            ins = f(e)
                        if inc is not None:
                            ins.then_inc(self.sems[inc[0]], inc[1])

                getattr(block, attr)(body)

    def close(self):
        while self.stacks:
            self.stacks.pop().close()
        self.es.close()

    def tt(self, eng, out, in0, in1, op, reads, writes):
        return self.op(eng, lambda e: e.tensor_tensor(out=out, in0=in0, in1=in1, op=op), reads, writes)

    def ts(self, eng, out, in0, s1, s2, op0, op1, reads, writes):
        if op1 is None:
            return self.op(eng, lambda e: e.tensor_scalar(out=out, in0=in0, scalar1=s1, scalar2=None, op0=op0),
                           reads, writes)
        return self.op(eng, lambda e: e.tensor_scalar(out=out, in0=in0, scalar1=s1, scalar2=s2, op0=op0, op1=op1),
                       reads, writes)

    def stt(self, out, in0, scalar, in1, op0, op1, reads, writes):
        return self.op("dve", lambda e: e.scalar_tensor_tensor(out=out, in0=in0, scalar=scalar, in1=in1,
                                                               op0=op0, op1=op1), reads, writes)

    def act(self, out, in_, func, reads, writes, **kw):
        return self.op("act", lambda e: e.activation(out=out, in_=in_, func=func, **kw), reads, writes)

    def cp(self, eng, out, in_, reads, writes):
        if eng == "act":
            return self.op("act", lambda e: e.copy(out=out, in_=in_), reads, writes)
        return self.op(eng, lambda e: e.tensor_copy(out=out, in_=in_), reads, writes)


class Rec:
    def __init__(self):
        self.calls = []

    def __getattr__(self, name):
        def f(*a, **kw):
            self.calls.append((name, a, kw))
        return f


def interleave(p, lists):
    def run(c):
        getattr(p, c[0])(*c[1], **c[2])
    lists = [(x if isinstance(x, tuple) else (x, len(x) // 2)) for x in lists]
    if True:
        for x, _ in lists:
            for c in x:
                run(c)
        return
    n = len(lists)
    if n == 0:
        return
    for c in lists[0][0][:lists[0][1]]:
        run(c)
    for i in range(n):
        a = lists[i][0][lists[i][1]:]
        b = lists[i + 1][0][:lists[i + 1][1]] if i + 1 < n else []
        for j in range(max(len(a), len(b))):
            if j < len(a):
                run(a[j])
            if j < len(b):
                run(b[j])


def make_consts(S):
    c = {}
    c["c_ident"] = np.eye(128, dtype=np.float32)
    half = 64
    inv_freq = (10000.0 ** (-np.arange(half, dtype=np.float32) / np.float32(half))).astype(np.float32)
    ang = (np.arange(S, dtype=np.float32)[:, None] * inv_freq[None, :]).astype(np.float32)
    cos = np.cos(ang).astype(np.float32)
    sin = np.sin(ang).astype(np.float32)
    c["c_rope"] = np.concatenate([cos, cos, -sin, sin], axis=1).astype(np.float32)
    p = np.arange(128)
    same = (p[:, None] // 64) == (p[None, :] // 64)
    tri = np.zeros((128, 4, 128), np.float32)
    tri[:, 0, :] = same & (p[:, None] <= p[None, :])
    tri[:, 1, :] = same & (p[:, None] >= p[None, :])
    tri[:, 2, :] = same & (p[:, None] > p[None, :])
    tri[:, 3, :] = same & (p[:, None] < p[None, :])
    c["c_tri"] = tri
    j = np.arange(64)
    m = np.zeros((64, 2, 8, 64), np.float32)
    m[:, 0, :, :] = (j[:, None] <= j[None, :])[:, None, :]
    m[:, 1, 0:4, :] = (j[:, None] > j[None, :])[:, None, :]
    m[:, 1, 4:8, :] = (j[:, None] >= j[None, :])[:, None, :]
    c["c_mask"] = m
    lg = np.log1p(-(2.0 ** (-5.0 - np.arange(4, dtype=np.float64))))
    jj = (p % 64).astype(np.float64)[:, None]
    sc = 128.0 ** -0.5
    rd = np.zeros((128, 6, 4), np.float64)
    rd[:, 0] = np.exp((jj + 1) * lg)
    rd[:, 1] = np.exp(-(jj + 1) * lg) * sc
    rd[:, 2] = np.exp((63 - jj) * lg) * sc
    rd[:, 3] = np.exp((64 - jj) * lg)
    rd[:, 4] = np.exp(-(64 - jj) * lg) * sc
    rd[:, 5] = np.exp(jj * lg) * sc
    c["c_rdec"] = rd.astype(np.float32)
    rt = np.stack([rd[:, 0], rd[:, 1], rd[:, 3], rd[:, 4]], 0)
    c["c_rtab"] = np.ascontiguousarray(rt.transpose(0, 2, 1)).reshape(1, 4 * 4 * 128).astype(np.float32)
    c["c_rcd"] = np.broadcast_to(np.exp(64 * lg)[None, :], (128, 4)).astype(np.float32).copy()
    tk_ = np.zeros((128, S // 128, 2), np.float32)
    tk_[:, :, 0] = np.arange(S // 128)[None, :]
    tk_[:, :, 1] = np.arange(128)[:, None]
    c["c_tok"] = tk_
    c["c_tstrict"] = (p[:, None] < p[None, :]).astype(np.float32)
    return c


CONST_SHAPES = {
    "c_ident": lambda S: [128, 128], "c_rope": lambda S: [S, 256], "c_tri": lambda S: [128, 4, 128], "c_rtab": lambda S: [1, 2048],
    "c_mask": lambda S: [64, 2, 8, 64], "c_rdec": lambda S: [128, 6, 4], "c_rcd": lambda S: [128, 4],
    "c_tstrict": lambda S: [128, 128], "c_tok": lambda S: [128, S // 128, 2],
}


import os
SKIP = set()


def build(S, L, dbg=False, stop_after=None):
    stop_layer = int(os.environ.get("STOPL", "0"))
    stop_req = stop_after
    NT = S // 128
    NC = S // 64
    CAP = 2 * S // NE
    KT = CAP // 128
    NS = min(512, CAP)
    NSH = CAP // NS
    nc = bass.Bass("TRN2", target_bir_lowering=False)
    p = Prog(nc, same_engine_sync=True)
    skind = "ExternalOutput" if dbg else "Internal"

    x_in = p.dram("x", [S, D], F32, kind="ExternalInput")
    W = {}
    for nm, shp in [("norm1_g", [L, D]), ("w_in", [L, D, INC]), ("ret_norm_g", [L, 512]),
                    ("hgrn_norm_g", [L, 512]), ("w_out", [L, D, D]), ("lower_bounds", [L, 512]),
                    ("norm2_g", [L, D]), ("w_router", [L, D, NE]), ("w_gate", [L, NE, D, D]),
                    ("w_up", [L, NE, D, D]), ("w_down", [L, NE, D, D]), ("final_norm_g", [1, D])]:
        W[nm] = p.dram(nm, shp, F32, kind="ExternalInput")
    C = {k: p.dram(k, f(S), F32, kind="ExternalInput") for k, f in CONST_SHAPES.items()}
    out_d = p.dram("out", [S, D], F32, kind="ExternalOutput")

    qkT_d = [p.dram("s_qkT%d" % d, [NT, 128, 2, 8, 128], BF16, kind=skind) for d in range(2)]
    khat_d = [p.dram("s_khat%d" % d, [S, D], BF16, kind=skind) for d in range(2)]
    vv_d = p.dram("s_vv", [S, D], BF16, kind=skind)
    gates_d = p.dram("s_gates", [S, D], BF16, kind=skind)
    o_d = [p.dram("s_o%d" % d, [S, D], F32, kind=skind) for d in range(2)]
    RW = 1088
    hbuf = [p.dram("s_h%d" % i, [S, D], F32, kind=skind) for i in range(2)]
    xn2_d = p.dram("s_xn2", [S, D], BF16, kind=skind)
    xe_d = [p.dram("s_xe%d" % e, [CAP, RW], BF16, kind=skind) for e in range(NE)]
    dbg_d = {}
    if dbg:
        dbg_d["aff"] = p.dram("s_aff", [128, NE, NT], F32, kind=skind)
        dbg_d["pos"] = p.dram("s_pos", [128, NE, NT], I32, kind=skind)
        dbg_d["thr"] = p.dram("s_thr", [128, NE], F32, kind=skind)
        dbg_d["dec0"] = p.dram("s_dec0", [128, NC, 8], F32, kind=skind)
        dbg_d["dec1"] = p.dram("s_dec1", [128, NC, 8], F32, kind=skind)

    gsem = p.dsems(2, "sw")
    identb = p.sb([128, 128], BF16, "identb")
    p.dma("pool", identb[:], C["c_ident"][:], gsem[0], writes=[identb])
    ones = p.sb([128, 128], F32, "ones")
    p.op("dve", lambda e: e.memset(ones[:], 1.0), writes=[ones])
    onesb = p.sb([128, 128], BF16, "onesb")
    p.op("dve", lambda e: e.memset(onesb[:], 1.0), writes=[onesb])
    onec = p.sb([128, 1], F32, "onec")
    p.op("dve", lambda e: e.memset(onec[:], 1.0), writes=[onec])
    epsc = p.sb([128, 1], F32, "epsc")
    p.op("dve", lambda e: e.memset(epsc[:], EPS), writes=[epsc])

    def rms_rstd(ssum, n, out_rstd, width=1):
        p.act(out_rstd, ssum, AF.Ln, [ssum_res(ssum), epsc], [ssum_res(out_rstd)], scale=1.0 / n, bias=epsc[:])
        p.act(out_rstd, out_rstd, AF.Exp, [ssum_res(out_rstd)], [ssum_res(out_rstd)], scale=-0.5)

    _apres = {}

    def ssum_res(ap):
        return _apres[id(ap)]

    def reg(ap, t):
        _apres[id(ap)] = t
        return ap

    _regc = {}

    def sreg(e):
        if "s" not in _regc:
            _regc["s"] = e.to_reg(S - 1)
        return _regc["s"]

    def bcreg(e):
        if "bc" not in _regc:
            _regc["bc"] = e.to_reg(CAP - 1)
        return _regc["bc"]

    def layer(l, h_cur):
        stop_after = stop_req if l == stop_layer else None
        last = (l == L - 1)
        h1_d = hbuf[l % 2]
        h_next = h1_d
        p.push()
        dec = [p.sb([128, NC, 8], F32, "dec%d" % d) for d in range(2)]

        p.push()
        sA = p.dsems(16)
        sAw = p.dsems(3, "sw")
        win = p.sb([128, 8, INC], BF16, "win")
        win_k = [Res("win%d" % k) for k in range(8)]
        for k in range(8):
            p.dma("pool", win[:, k, :], W["w_in"][l, k * 128:(k + 1) * 128, :], sAw[k % 2], writes=[win_k[k]])
        g1b = p.sb([128, D], F32, "g1b")
        p.dma("sp", g1b[:], W["norm1_g"][l:l + 1, :].partition_broadcast(128), sA[2], writes=[g1b])
        tri = p.sb([128, 4, 128], BF16, "tri")
        p.dma("pool", tri[:], C["c_tri"][:], sAw[2], writes=[tri])
        rdec = p.sb([128, 6, 4], F32, "rdec")
        p.dma("sp", rdec[:], C["c_rdec"][:], sA[2], writes=[rdec])
        rcd = p.sb([128, 4], F32, "rcd")
        p.dma("sp", rcd[:], C["c_rcd"][:], sA[3], writes=[rcd])
        tabT = p.sb([128, 4, 4, 128], F32, "tabT")
        p.dma("sp", tabT[:].rearrange("p a h t -> p (a h t)"), C["c_rtab"][0:1, :].partition_broadcast(128), sA[2],
              writes=[tabT])
        for d in range(2):
            p.cp("dve", dec[d][:, :, 0:4], rcd[:].unsqueeze(1).to_broadcast([128, NC, 4]), [rcd], [dec[d]])
        use_lb = l > 0
        sgn = 1.0 if use_lb else -1.0
        if use_lb:
            assert l == 1
            lbv = p.sb([128, 512], F32, "lbv")
            oml = p.sb([128, 512], F32, "oml")
            p.dma("sp", lbv[:], W["lower_bounds"][1:2, :].partition_broadcast(128), sA[2], writes=[lbv])
            p.dma("sp", oml[:], W["lower_bounds"][0:1, :].partition_broadcast(128), sA[3], writes=[oml])
            p.tt("dve", oml[:], oml[:], lbv[:], ALU.subtract, [oml, lbv], [oml])
            p.act(oml[:], oml[:], AF.Exp, [oml], [oml])
            p.ts("dve", oml[:], oml[:], 1.0, None, ALU.add, None, [oml], [oml])
            p.op("dve", lambda e: e.reciprocal(out=lbv[:], in_=oml[:]), [oml], [lbv])
            p.ts("dve", oml[:], lbv[:], -1.0, 1.0, ALU.mult, ALU.add, [lbv], [oml])

        def dbl(shape, dt, nm):
            return [p.sb(shape, dt, "%s%d" % (nm, i)) for i in range(2)]

        xt = dbl([128, D], F32, "xt")
        rope = dbl([128, 256], F32, "rope")
        ss = dbl([128, 1], F32, "ss")
        rstd = dbl([128, 1], F32, "rstd")
        xn = dbl([128, D], BF16, "xn")
        xnT = dbl([128, 8, 128], BF16, "xnT")
        zlo = dbl([128, 8], F32, "zlo")
        q2, te, tf, tk, tcm, qa, qb, krot = [dbl([128, 512], F32, n) for n in
                                             ("q2", "te", "tf", "tk", "tcm", "qa", "qb", "krot")]
        tE1, tE2, tE3, thi, tlo = [dbl([128, 512], BF16, n) for n in ("tE1", "tE2", "tE3", "thi", "tlo")]
        tokQr = dbl([128, 512], BF16, "tokQr")
        tokKr = dbl([128, 512], BF16, "tokKr")
        hgQ = [dbl([128, 512], BF16, "hgQ%d" % d) for d in range(2)]
        hgK = [dbl([128, 512], BF16, "hgK%d" % d) for d in range(2)]
        khat = [dbl([128, D], BF16, "khat%d" % d) for d in range(2)]
        vtile = dbl([128, D], BF16, "vtile")
        gtile = dbl([128, D], BF16, "gtile")
        qkTs = [dbl([128, 2, 8, 128], BF16, "qkTs%d" % d) for d in range(2)]
        pT = p.ps([128, 8, 128], BF16, "pT")
        pG = [p.ps([128, 512], F32, "pG%d" % i) for i in range(2)]
        pC = p.ps([128, 2, 512], F32, "pC")
        pX = [p.ps([128, 8, 128], BF16, "pX%d" % i) for i in range(2)]
        pZ = p.ps([128, 512], F32, "pZ")
        rot = {"g": 0, "x": 0}

        def tile_ops(t):
            q = Rec()
            b = t % 2
            xb, rp = xt[b], rope[b]
            q.dma("sp", xb[:], h_cur[t * 128:(t + 1) * 128, :], sA[4 + b], writes=[xb])
            q.dma("sp", rp[:], C["c_rope"][t * 128:(t + 1) * 128, :], sA[6 + b], writes=[rp])
            q.act(xn[b][:], xb[:], AF.Square, [xb], [xn[b], ss[b]], accum_out=ss[b][:])
            q.act(rstd[b][:], ss[b][:], AF.Ln, [ss[b], epsc], [rstd[b]], scale=1.0 / D, bias=epsc[:])
            q.act(rstd[b][:], rstd[b][:], AF.Exp, [rstd[b]], [rstd[b]], scale=-0.5)
            q.stt(xn[b][:], xb[:], rstd[b][:], g1b[:], ALU.mult, ALU.mult, [xb, rstd[b], g1b], [xn[b]])
            q.op("pe", [lambda e, k=k: e.transpose(out=pT[:, k, :], in_=xn[b][:, k * 128:(k + 1) * 128],
                                                   identity=identb[:]) for k in range(8)], [xn[b], identb], [pT])
            q.cp("act", xnT[b][:], pT[:], [pT], [xnT[b]])

            def group(g):
                pg = pG[rot["g"] % 2]
                rot["g"] += 1
                q.op("pe", [lambda e, k=k: e.matmul(pg[:], lhsT=xnT[b][:, k, :], rhs=win[:, k, g * 512:(g + 1) * 512],
                                                    start=(k == 0), stop=(k == 7)) for k in range(8)],
                     [xnT[b]] + win_k, [pg])
                return pg

            def sig_chain(pg):
                q.act(te[b][:], pg[:], AF.Exp, [pg], [te[b]], scale=-1.0)
                q.act(te[b][:], te[b][:], AF.Ln, [te[b]], [te[b]], bias=onec[:])
                q.act(tf[b][:], te[b][:], AF.Exp, [te[b]], [tf[b]], scale=-1.0)

            def rotary(pg, dst_t, dst_ap):
                pg4 = pg[:].rearrange("p (h d) -> p h d", h=4)
                cs = rp[:, 0:128].unsqueeze(1).to_broadcast([128, 4, 128])
                sn1 = rp[:, 128:192].unsqueeze(1).to_broadcast([128, 4, 64])
                sn2 = rp[:, 192:256].unsqueeze(1).to_broadcast([128, 4, 64])
                qa4 = qa[b][:].rearrange("p (h d) -> p h d", h=4)
                qb4 = qb[b][:].rearrange("p (h d) -> p h d", h=4)
                q.tt("dve", qa4, pg4, cs, ALU.mult, [pg, rp], [qa[b]])
                q.op("dve", [lambda e: e.tensor_tensor(out=qb4[:, :, 0:64], in0=pg4[:, :, 64:128], in1=sn1, op=ALU.mult),
                             lambda e: e.tensor_tensor(out=qb4[:, :, 64:128], in0=pg4[:, :, 0:64], in1=sn2, op=ALU.mult)],
                     [pg, rp], [qb[b]])
                q.tt("dve", dst_ap, qa[b][:], qb[b][:], ALU.add, [qa[b], qb[b]], [dst_t])

            def forget(pg, d):
                sig_chain(pg)
                if use_lb:
                    lbs_ = ""
                    if "m" not in lbs_:
                        q.tt("dve", tf[b][:], tf[b][:], oml[:], ALU.mult, [tf[b], oml], [tf[b]])
                        q.tt("dve", tf[b][:], tf[b][:], lbv[:], ALU.add, [tf[b], lbv], [tf[b]])
                    if "c" in lbs_:
                        q.act(te[b][:], tf[b][:], AF.Identity, [tf[b]], [te[b]], scale=-0.5)
                    elif "l" not in lbs_:
                        q.act(te[b][:], tf[b][:], AF.Ln, [tf[b]], [te[b]])
                q.ts("dve", tk[b][:], tf[b][:], -1.0, 1.0, ALU.mult, ALU.add, [tf[b]], [tk[b]])
                q.cp("act", thi[b][:], te[b][:], [te[b]], [thi[b]])
                q.tt("dve", tlo[b][:], te[b][:], thi[b][:], ALU.subtract, [te[b], thi[b]], [tlo[b]])
                q.op("pe", [lambda e: e.matmul(pC[:, 0, :], lhsT=tri[:, d, :], rhs=thi[b][:], start=True, stop=False),
                            lambda e: e.matmul(pC[:, 0, :], lhsT=tri[:, d, :], rhs=tlo[b][:], start=False, stop=True),
                            lambda e: e.matmul(pC[:, 1, :], lhsT=tri[:, 2 + d, :], rhs=thi[b][:], start=True, stop=False),
                            lambda e: e.matmul(pC[:, 1, :], lhsT=tri[:, 2 + d, :], rhs=tlo[b][:], start=False, stop=True)],
                     [tri, thi[b], tlo[b]], [pC])
                fns = []
                for h in range(4):
                    for c2 in range(2):
                        for part, src in enumerate((thi[b], tlo[b])):
                            col = part * 8 + h * 2 + c2
                            fns.append(lambda e, h=h, c2=c2, col=col, src=src: e.matmul(
                                pZ[:, col:col + 1], lhsT=src[c2 * 64:(c2 + 1) * 64, h * 128:(h + 1) * 128],
                                rhs=onesb[c2 * 64:(c2 + 1) * 64, 0:1], start=True, stop=True))
                q.op("pe", fns, [thi[b], tlo[b], onesb], [pZ])
                q.cp("act", zlo[b][:], pZ[:, 8:16], [pZ], [zlo[b]])
                q.tt("dve", zlo[b][:], pZ[:, 0:8], zlo[b][:], ALU.add, [pZ, zlo[b]], [zlo[b]])
                q.act(dec[d][:, 2 * t:2 * t + 2, 4:8], zlo[b][:].rearrange("p (h c) -> p c h", c=2), AF.Exp,
                      [zlo[b]], [dec[d]], scale=sgn)
                if use_lb:
                    q.ts("dve", tcm[b][:], pC[:, 0, :], -80.0, None, ALU.max, None, [pC], [tcm[b]])
                else:
                    q.ts("dve", tcm[b][:], pC[:, 0, :], 80.0, None, ALU.min, None, [pC], [tcm[b]])
                q.act(tE1[b][:], tcm[b][:], AF.Exp, [tcm[b]], [tE1[b]], scale=sgn)
                q.act(tE2[b][:], tcm[b][:], AF.Exp, [tcm[b]], [tE2[b]], scale=-sgn)
                q.act(tE3[b][:], pC[:, 1, :], AF.Exp, [pC], [tE3[b]], scale=sgn)
                q.stt(hgQ[d][b][:], q2[b][:], 128.0 ** -0.5, tE1[b][:], ALU.mult, ALU.mult, [q2[b], tE1[b]], [hgQ[d][b]])
                q.tt("dve", hgK[d][b][:], tk[b][:], tE2[b][:], ALU.mult, [tk[b], tE2[b]], [hgK[d][b]])
                q.tt("dve", khat[d][b][:, 512:1024], tk[b][:], tE3[b][:], ALU.mult, [tk[b], tE3[b]], [khat[d][b]])

            pg = group(4)
            sig_chain(pg)
            q.tt("dve", q2[b][:], pg[:], tf[b][:], ALU.mult, [pg, tf[b]], [q2[b]])
            pg = group(5)
            forget(pg, 0)
            pg = group(6)
            forget(pg, 1)
            split = len(q.calls)
            pg = group(0)
            rotary(pg, tokQr[b], tokQr[b][:])
            pg = group(1)
            rotary(pg, krot[b], krot[b][:])
            q.cp("act", tokKr[b][:], krot[b][:], [krot[b]], [tokKr[b]])
            for d, ti in ((0, 2), (1, 5)):
                tb = rdec[:, ti, :].unsqueeze(2).to_broadcast([128, 4, 128])
                q.tt("dve", khat[d][b][:, 0:512].rearrange("p (h d) -> p h d", h=4),
                     krot[b][:].rearrange("p (h d) -> p h d", h=4), tb, ALU.mult, [krot[b], rdec], [khat[d][b]])
            pg = group(3)
            sig_chain(pg)
            q.tt("dve", gtile[b][:, 0:512], pg[:], tf[b][:], ALU.mult, [pg, tf[b]], [gtile[b]])
            pg = group(8)
            sig_chain(pg)
            q.tt("dve", gtile[b][:, 512:1024], pg[:], tf[b][:], ALU.mult, [pg, tf[b]], [gtile[b]])
            pg = group(2)
            q.cp("act", vtile[b][:, 0:512], pg[:], [pg], [vtile[b]])
            pg = group(7)
            q.cp("act", vtile[b][:, 512:1024], pg[:], [pg], [vtile[b]])
            for d in range(2):
                for wi in range(2):
                    px = pX[rot["x"] % 2]
                    rot["x"] += 1
                    sr = tokQr[b] if wi == 0 else tokKr[b]
                    sh = hgQ[d][b] if wi == 0 else hgK[d][b]
                    fns = [lambda e, h=h, sr=sr, px=px: e.transpose(out=px[:, h, :], in_=sr[:, h * 128:(h + 1) * 128],
                                                                   identity=identb[:]) for h in range(4)]
                    fns += [lambda e, h=h, sh=sh, px=px: e.transpose(out=px[:, 4 + h, :], in_=sh[:, h * 128:(h + 1) * 128],
                                                                   identity=identb[:]) for h in range(4)]
                    q.op("pe", fns, [sr, sh, identb], [px])
                    q.tt("dve", qkTs[d][b][:, wi, 0:4, :], px[:, 0:4, :], tabT[:, 2 * d + wi, :, :], ALU.mult,
                         [px, tabT], [qkTs[d][b]])
                    q.cp("act", qkTs[d][b][:, wi, 4:8, :], px[:, 4:8, :], [px], [qkTs[d][b]])
                q.dma("sp", qkT_d[d][t], qkTs[d][b][:], sA[8 + d], reads=[qkTs[d][b]], writes=[qkT_d[d]])
                q.dma("sp", khat_d[d][t * 128:(t + 1) * 128, :], khat[d][b][:], sA[10 + d], reads=[khat[d][b]],
                      writes=[khat_d[d]])
            q.dma("sp", vv_d[t * 128:(t + 1) * 128, :], vtile[b][:], sA[12], reads=[vtile[b]], writes=[vv_d])
            q.dma("sp", gates_d[t * 128:(t + 1) * 128, :], gtile[b][:], sA[13], reads=[gtile[b]], writes=[gates_d])
            return q.calls, split

        interleave(p, [tile_ops(t) for t in range(NT)])
        if dbg:
            for d in range(2):
                p.dma("sp", dbg_d["dec%d" % d][:], dec[d][:], sA[14 + d], reads=[dec[d]], writes=[dbg_d["dec%d" % d]])
        p.pop()
        if stop_after == "A":
            return None

        p.push()
        sF = p.dsems(16)
        mask = p.sb([64, 2, 8, 64], F32, "mask")
        p.dma("sp", mask[:], C["c_mask"][:], sF[0], writes=[mask])
        Sst = [p.sb([128, 8, 128], F32, "Sst%d" % d) for d in range(2)]
        Sbf = [p.sb([128, 8, 128], BF16, "Sbf%d" % d) for d in range(2)]
        for d in range(2):
            p.op("dve", lambda e, d=d: e.memset(Sst[d][:], 0.0), writes=[Sst[d]])
            p.op("pool", lambda e, d=d: e.memset(Sbf[d][:], 0.0), writes=[Sbf[d]])
        qkb = [[p.sb([128, 2, 8, 128], BF16, "qkb%d_%d" % (d, i)) for i in range(2)] for d in range(2)]
        khb = [[p.sb([64, D], BF16, "khb%d_%d" % (d, i)) for i in range(2)] for d in range(2)]
        vvb = [[p.sb([64, D], BF16, "vvb%d_%d" % (d, i)) for i in range(2)] for d in range(2)]
        smb = [p.sb([64, 8, 64], BF16, "smb%d" % d) for d in range(2)]
        osb = [[p.sb([64, D], F32, "osb%d_%d" % (d, i)) for i in range(2)] for d in range(2)]
        pS = [p.ps([64, 8, 64], F32, "pS%d" % d) for d in range(2)]
        pO = [p.ps([64, D], F32, "pO%d" % d) for d in range(2)]
        pD = p.ps([128, 8, 128], F32, "pD")

        def scan_loads(i, d):
            c = i if d == 0 else NC - 1 - i
            t, half = c // 2, c % 2
            first_half = (half == 0) if d == 0 else (half == 1)
            if first_half:
                qb_ = qkb[d][(i // 2) % 2]
                p.dma("sp", qb_[:], qkT_d[d][t], sF[1 + d * 2 + (i // 2) % 2], reads=[qkT_d[d]], writes=[qb_])
            kb_ = khb[d][i % 2]
            vb_ = vvb[d][i % 2]
            p.dma("sp", kb_[:], khat_d[d][c * 64:(c + 1) * 64, :], sF[5 + d * 2 + i % 2], reads=[khat_d[d]], writes=[kb_])
            p.dma("sp", vb_[:], vv_d[c * 64:(c + 1) * 64, :], sF[9 + d * 2 + i % 2], reads=[vv_d], writes=[vb_])

        for d in range(2):
            scan_loads(0, d)
        for i in range(NC):
            for d in range(2):
                if i + 1 < NC:
                    scan_loads(i + 1, d)
                c = i if d == 0 else NC - 1 - i
                half = c % 2
                qb_ = qkb[d][(i // 2) % 2]
                kb_ = khb[d][i % 2]
                vb_ = vvb[d][i % 2]
                cs = slice(half * 64, (half + 1) * 64)
                p.op("pe", [lambda e, h=h, d=d, qb_=qb_, cs=cs: e.matmul(
                    pS[d][:, h, :], lhsT=qb_[:, 1, h, cs], rhs=qb_[:, 0, h, cs], start=True, stop=True)
                            for h in range(8)], [qb_], [pS[d]])
                p.tt("dve", smb[d][:], pS[d][:], mask[:, d, :, :], ALU.mult, [pS[d], mask], [smb[d]])
                fns = []
                for h in range(8):
                    hs = slice(h * 128, (h + 1) * 128)
                    fns.append(lambda e, h=h, hs=hs, d=d, vb_=vb_: e.matmul(
                        pO[d][:, hs], lhsT=smb[d][:, h, :], rhs=vb_[:, hs], start=True, stop=False))
                    fns.append(lambda e, h=h, hs=hs, d=d, qb_=qb_, cs=cs: e.matmul(
                        pO[d][:, hs], lhsT=qb_[:, 0, h, cs], rhs=Sbf[d][:, h, :], start=False, stop=True))
                p.op("pe", fns, [smb[d], vb_, qb_, Sbf[d]], [pO[d]])
                ob_ = osb[d][i % 2]
                p.cp("act", ob_[:], pO[d][:], [pO[d]], [ob_])
                p.dma("sp", o_d[d][c * 64:(c + 1) * 64, :], ob_[:], sF[13 + d], reads=[ob_], writes=[o_d[d]])
                if i + 1 < NC:
                    p.op("pe", [lambda e, h=h, kb_=kb_, vb_=vb_: e.matmul(
                        pD[:, h, :], lhsT=kb_[:, h * 128:(h + 1) * 128], rhs=vb_[:, h * 128:(h + 1) * 128],
                        start=True, stop=True) for h in range(8)], [kb_, vb_], [pD])
                    p.op("dve", [lambda e, h=h, d=d, c=c: e.scalar_tensor_tensor(
                        out=Sst[d][:, h, :], in0=Sst[d][:, h, :], scalar=dec[d][:, c, h:h + 1], in1=pD[:, h, :],
                        op0=ALU.mult, op1=ALU.add) for h in range(8)], [Sst[d], dec[d], pD], [Sst[d]])
                    p.cp("act", Sbf[d][:], Sst[d][:], [Sst[d]], [Sbf[d]])
        p.pop()
        p.pop()
        if stop_after == "F":
            return None

        p.push()
        affT = p.sb([128, NE, NT], F32, "affT")
        gate_m = p.sb([128, NE, NT], F32, "gate_m")
        pos_i = p.sb([128, NE, NT], I32, "pos_i")

        p.push()
        sG = p.dsems(16)
        sGw = p.dsems(1, "sw")
        wout = p.sb([128, 8, D], BF16, "wout")
        p.dma("pool", wout[:], W["w_out"][l].rearrange("(k p) n -> p k n", p=128), sGw[0], writes=[wout])
        wrf = p.sb([128, 8, NE], F32, "wrf")
        p.dma("sp", wrf[:], W["w_router"][l].rearrange("(k p) n -> p k n", p=128), sG[1], writes=[wrf])
        wr = p.sb([128, 8, NE], BF16, "wr")
        wrl = p.sb([128, 8, NE], BF16, "wrl")
        p.cp("dve", wr[:], wrf[:], [wrf], [wr])
        p.tt("dve", wrl[:], wrf[:], wr[:], ALU.subtract, [wrf, wr], [wrl])
        gmix = p.sb([128, D], F32, "gmix")
        p.dma("sp", gmix[:, 0:512], W["ret_norm_g"][l:l + 1, :].partition_broadcast(128), sG[2], writes=[gmix])
        p.dma("sp", gmix[:, 512:1024], W["hgrn_norm_g"][l:l + 1, :].partition_broadcast(128), sG[2], writes=[gmix])
        g2b = p.sb([128, D], F32, "g2b")
        p.dma("sp", g2b[:], W["norm2_g"][l:l + 1, :].partition_broadcast(128), sG[3], writes=[g2b])
        ofb = [p.sb([128, D], F32, "ofb%d" % i) for i in range(2)]
        obb = [p.sb([128, D], F32, "obb%d" % i) for i in range(2)]
        gtb = [p.sb([128, D], BF16, "gtb%d" % i) for i in range(2)]
        hb = [p.sb([128, D], F32, "hb%d" % i) for i in range(2)]
        osum = p.sb([128, D], F32, "osum")
        gg = p.sb([128, D], F32, "gg")
        junkf = p.sb([128, D], BF16, "junkf")
        ss5 = p.sb([128, 8], F32, "ss5")
        rs5 = p.sb([128, 8], F32, "rs5")
        mixed = p.sb([128, D], BF16, "mixed")
        mT = p.sb([128, 8, 128], BF16, "mT")
        h1b = [p.sb([128, D], F32, "h1b%d" % i) for i in range(2)]
        xn2b = [p.sb([128, D], BF16, "xn2b%d" % i) for i in range(2)]
        x2T = p.sb([128, 8, 128], BF16, "x2T")
        x2Tl = p.sb([128, 8, 128], BF16, "x2Tl")
        xn2f = p.sb([128, D], F32, "xn2f")
        xn2l = p.sb([128, D], BF16, "xn2l")
        pT4 = p.ps([128, 8, 128], BF16, "pT4")
        ss2 = p.sb([128, 1], F32, "ss2")
        rs2 = p.sb([128, 1], F32, "rs2")
        mx = p.sb([128, 1], F32, "mx")
        sme = p.sb([128, 1], F32, "sme")
        ex = p.sb([128, NE], F32, "ex")
        pT2 = p.ps([128, 8, 128], BF16, "pT2")
        pT3 = p.ps([128, 8, 128], BF16, "pT3")
        pH = p.ps([128, D], F32, "pH")
        pL = p.ps([128, 512], F32, "pL")

        def g_loads(t):
            r = slice(t * 128, (t + 1) * 128)
            p.dma("sp", ofb[t % 2][:], o_d[0][r, :], sG[4 + t % 2], reads=[o_d[0]], writes=[ofb[t % 2]])
            p.dma("sp", obb[t % 2][:], o_d[1][r, :], sG[6 + t % 2], reads=[o_d[1]], writes=[obb[t % 2]])
            p.dma("sp", gtb[t % 2][:], gates_d[r, :], sG[8 + t % 2], reads=[gates_d], writes=[gtb[t % 2]])
            p.dma("sp", hb[t % 2][:], h_cur[r, :], sG[10 + t % 2], reads=[h_cur], writes=[hb[t % 2]])

        g_loads(0)
        for t in range(NT):
            if t + 1 < NT:
                g_loads(t + 1)
            r = slice(t * 128, (t + 1) * 128)
            of_, ob_, gt_, h_ = ofb[t % 2], obb[t % 2], gtb[t % 2], hb[t % 2]
            p.tt("dve", osum[:], of_[:], ob_[:], ALU.add, [of_, ob_], [osum])
            p.tt("dve", gg[:], gt_[:], gmix[:], ALU.mult, [gt_, gmix], [gg])
            fns = [lambda e, h=h: e.activation(out=junkf[:, h * 128:(h + 1) * 128], in_=osum[:, h * 128:(h + 1) * 128],
                                               func=AF.Square, accum_out=ss5[:, h:h + 1]) for h in range(4)]
            fns.append(lambda e: e.activation(out=junkf[:, 512:1024], in_=osum[:, 512:1024], func=AF.Square,
                                              accum_out=ss5[:, 4:5]))
            p.op("act", fns, [osum], [junkf, ss5])
            p.act(rs5[:, 0:4], ss5[:, 0:4], AF.Ln, [ss5, epsc], [rs5], scale=1.0 / 128, bias=epsc[:])
            p.act(rs5[:, 4:5], ss5[:, 4:5], AF.Ln, [ss5, epsc], [rs5], scale=1.0 / 512, bias=epsc[:])
            p.act(rs5[:, 0:5], rs5[:, 0:5], AF.Exp, [rs5], [rs5], scale=-0.5)
            fns = [lambda e, h=h: e.scalar_tensor_tensor(out=mixed[:, h * 128:(h + 1) * 128],
                                                         in0=osum[:, h * 128:(h + 1) * 128], scalar=rs5[:, h:h + 1],
                                                         in1=gg[:, h * 128:(h + 1) * 128], op0=ALU.mult, op1=ALU.mult)
                   for h in range(4)]
            fns.append(lambda e: e.scalar_tensor_tensor(out=mixed[:, 512:1024], in0=osum[:, 512:1024],
                                                        scalar=rs5[:, 4:5], in1=gg[:, 512:1024],
                                                        op0=ALU.mult, op1=ALU.mult))
            p.op("dve", fns, [osum, rs5, gg], [mixed])
            p.op("pe", [lambda e, k=k: e.transpose(out=pT2[:, k, :], in_=mixed[:, k * 128:(k + 1) * 128],
                                                   identity=identb[:]) for k in range(8)], [mixed, identb], [pT2])
            p.cp("act", mT[:], pT2[:], [pT2], [mT])
            fns = []
            for n2 in range(2):
                for k in range(8):
                    fns.append(lambda e, k=k, n2=n2: e.matmul(pH[:, n2 * 512:(n2 + 1) * 512], lhsT=mT[:, k, :],
                                                            rhs=wout[:, k, n2 * 512:(n2 + 1) * 512],
                                                            start=(k == 0), stop=(k == 7)))
            p.op("pe", fns, [mT, wout], [pH])
            h1_ = h1b[t % 2]
            p.tt("dve", h1_[:], pH[:], h_[:], ALU.add, [pH, h_], [h1_])
            p.dma("sp", h1_d[r, :], h1_[:], sG[12], reads=[h1_], writes=[h1_d])
            p.act(junkf[:], h1_[:], AF.Square, [h1_], [junkf, ss2], accum_out=ss2[:])
            p.act(rs2[:], ss2[:], AF.Ln, [ss2, epsc], [rs2], scale=1.0 / D, bias=epsc[:])
            p.act(rs2[:], rs2[:], AF.Exp, [rs2], [rs2], scale=-0.5)
            xn2_ = xn2b[t % 2]
            p.stt(xn2f[:], h1_[:], rs2[:], g2b[:], ALU.mult, ALU.mult, [h1_, rs2, g2b], [xn2f])
            p.cp("act", xn2_[:], xn2f[:], [xn2f], [xn2_])
            p.tt("dve", xn2l[:], xn2f[:], xn2_[:], ALU.subtract, [xn2f, xn2_], [xn2l])
            p.dma("sp", xn2_d[r, :], xn2_[:], sG[13], reads=[xn2_], writes=[xn2_d])
            p.op("pe", [lambda e, k=k, xn2_=xn2_: e.transpose(out=pT3[:, k, :], in_=xn2_[:, k * 128:(k + 1) * 128],
                                                            identity=identb[:]) for k in range(8)],
                 [xn2_, identb], [pT3])
            p.cp("dve", x2T[:], pT3[:], [pT3], [x2T])
            p.op("pe", [lambda e, k=k: e.transpose(out=pT4[:, k, :], in_=xn2l[:, k * 128:(k + 1) * 128],
                                                   identity=identb[:]) for k in range(8)],
                 [xn2l, identb], [pT4])
            p.cp("act", x2Tl[:], pT4[:], [pT4], [x2Tl])
            fns = []
            for part, (xa, wa) in enumerate(((x2T, wr), (x2Tl, wr), (x2T, wrl))):
                for k in range(8):
                    fns.append(lambda e, k=k, xa=xa, wa=wa, part=part: e.matmul(
                        pL[:, 0:NE], lhsT=xa[:, k, :], rhs=wa[:, k, :], start=(part == 0 and k == 0),
                        stop=(part == 2 and k == 7)))
            p.op("pe", fns, [x2T, x2Tl, wr, wrl], [pL])
            p.op("dve", lambda e: e.tensor_reduce(out=mx[:], in_=pL[:, 0:NE], axis=AX.X, op=ALU.max), [pL], [mx])
            p.ts("dve", mx[:], mx[:], -1.0, None, ALU.mult, None, [mx], [mx])
            p.act(ex[:], pL[:, 0:NE], AF.Exp, [pL, mx], [ex, sme], bias=mx[:], accum_out=sme[:])
            p.op("dve", lambda e: e.reciprocal(out=sme[:], in_=sme[:]), [sme], [sme])
            p.ts("dve", affT[:, :, t], ex[:], sme[:], None, ALU.mult, None, [ex, sme], [affT])
        p.pop()
        if stop_after == "G":
            return None

        p.push()
        sR = p.dsems(4)
        sRw = p.dsems(1, "sw")
        lo = p.sb([128, NE], F32, "lo")
        hi = p.sb([128, NE], F32, "hi")
        mid = p.sb([128, NE], F32, "mid")
        cmpb = p.sb([128, NE, NT], F32, "cmpb")
        cntp = p.sb([128, NE], F32, "cntp")
        cntpb = p.sb([128, NE], BF16, "cntpb")
        geu = p.sb([128, NE], U32, "geu")
        ltu = p.sb([128, NE], U32, "ltu")
        pcnt = p.ps([128, 512], F32, "pcnt")
        tstr = p.sb([128, 128], BF16, "tstr")
        p.dma("pool", tstr[:], C["c_tstrict"][:], sRw[0], writes=[tstr])
        p.op("dve", lambda e: e.memset(lo[:], 0.0), writes=[lo])
        p.op("dve", lambda e: e.memset(hi[:], 1.0), writes=[hi])
        for it in range(30):
            p.tt("dve", mid[:], lo[:], hi[:], ALU.add, [lo, hi], [mid])
            p.ts("dve", mid[:], mid[:], 0.5, None, ALU.mult, None, [mid], [mid])
            p.tt("dve", cmpb[:], affT[:], mid[:].unsqueeze(2).to_broadcast([128, NE, NT]), ALU.is_ge,
                 [affT, mid], [cmpb])
            p.op("dve", lambda e: e.tensor_reduce(out=cntp[:], in_=cmpb[:], axis=AX.X, op=ALU.add), [cmpb], [cntp])
            p.cp("dve", cntpb[:], cntp[:], [cntp], [cntpb])
            p.op("pe", lambda e: e.matmul(pcnt[:, 0:NE], lhsT=onesb[:], rhs=cntpb[:], start=True, stop=True),
                 [onesb, cntpb], [pcnt])
            p.ts("dve", geu[:], pcnt[:, 0:NE], float(CAP), None, ALU.is_ge, None, [pcnt], [geu])
            p.ts("dve", ltu[:], pcnt[:, 0:NE], float(CAP), None, ALU.is_lt, None, [pcnt], [ltu])
            p.op("dve", lambda e: e.copy_predicated(out=lo[:], mask=geu[:], data=mid[:]), [geu, mid], [lo])
            p.op("dve", lambda e: e.copy_predicated(out=hi[:], mask=ltu[:], data=mid[:]), [ltu, mid], [hi])
        msel = p.sb([128, NE, NT], F32, "msel")
        mselb = p.sb([128, NE, NT], BF16, "mselb")
        posf = p.sb([128, NE, NT], F32, "posf")
        tot = p.sb([128, NE, NT], F32, "tot")
        inc = p.sb([128, NE, NT], F32, "inc")
        p.tt("dve", msel[:], affT[:], lo[:].unsqueeze(2).to_broadcast([128, NE, NT]), ALU.is_ge, [affT, lo], [msel])
        p.cp("dve", mselb[:], msel[:], [msel], [mselb])
        NF = NE * NT
        pw = p.ps([128, max(NF, 512)], F32, "pw")
        pt_ = p.ps([128, max(NF, 512)], F32, "pt")
        mflat = mselb[:].rearrange("p e t -> p (e t)")
        fns = []
        for j in range(0, NF, 512):
            w = min(512, NF - j)
            fns.append(lambda e, j=j, w=w: e.matmul(pw[:, j:j + w], lhsT=tstr[:], rhs=mflat[:, j:j + w],
                                                    start=True, stop=True))
            fns.append(lambda e, j=j, w=w: e.matmul(pt_[:, j:j + w], lhsT=onesb[:], rhs=mflat[:, j:j + w],
                                                    start=True, stop=True))
        p.op("pe", fns, [tstr, onesb, mselb], [pw, pt_])
        p.cp("dve", tot[:].rearrange("p e t -> p (e t)"), pt_[:, 0:NF], [pt_], [tot])
        p.op("dve", [lambda e, ee=ee: e.tensor_tensor_scan(out=inc[:, ee, :], data0=ones[:, 0:NT], data1=tot[:, ee, :],
                                                           initial=0.0, op0=ALU.mult, op1=ALU.add)
                     for ee in range(NE)], [ones, tot], [inc])
        p.tt("dve", inc[:], inc[:], tot[:], ALU.subtract, [inc, tot], [inc])
        p.tt("dve", posf[:].rearrange("p e t -> p (e t)"), pw[:, 0:NF], inc[:].rearrange("p e t -> p (e t)"),
             ALU.add, [pw, inc], [posf])
        p.ts("dve", tot[:], posf[:], float(CAP), None, ALU.is_lt, None, [posf], [tot])
        p.tt("dve", msel[:], msel[:], tot[:], ALU.mult, [msel, tot], [msel])
        p.tt("dve", gate_m[:], affT[:], msel[:], ALU.mult, [affT, msel], [gate_m])
        p.ts("dve", tot[:], msel[:], -1.0e6, 1.0e6, ALU.mult, ALU.add, [msel], [tot])
        p.tt("dve", posf[:], posf[:], tot[:], ALU.add, [posf, tot], [posf])
        p.cp("dve", pos_i[:], posf[:], [posf], [pos_i])
        if dbg:
            p.dma("sp", dbg_d["aff"][:], affT[:], sR[1], reads=[affT], writes=[dbg_d["aff"]])
            p.dma("sp", dbg_d["pos"][:], pos_i[:], sR[2], reads=[pos_i], writes=[dbg_d["pos"]])
            p.dma("sp", dbg_d["thr"][:], lo[:], sR[3], reads=[lo], writes=[dbg_d["thr"]])
        p.pop()
        if stop_after == "R":
            return None

        p.push()
        sX = p.dsems(2) + p.dsems(8, "sw")
        xb2 = [p.sb([128, RW], BF16, "xb2_%d" % i) for i in range(2)]
        tok_i = p.sb([128, NT], I32, "tok_i")
        p.op("pool", lambda e: e.iota(tok_i[:], pattern=[[128, NT]], base=0, channel_multiplier=1), writes=[tok_i])
        for i in range(2):
            p.op("dve", lambda e, i=i: e.memset(xb2[i][:, 1024:RW], 0.0), writes=[xb2[i]])
        k = 0
        for t in range(NT):
            xb_ = xb2[t % 2]
            p.dma("sp", xb_[:, 0:1024], xn2_d[t * 128:(t + 1) * 128, :], sX[t % 2], reads=[xn2_d], writes=[xb_])
            p.cp("dve", xb_[:, 1024:1040], gate_m[:, :, t], [gate_m], [xb_])
            p.tt("dve", xb_[:, 1040:1056], gate_m[:, :, t], xb_[:, 1024:1040], ALU.subtract, [gate_m, xb_], [xb_])
            p.cp("dve", xb_[:, 1056:1058].bitcast(I32), tok_i[:, t:t + 1], [tok_i], [xb_])
            for ee in range(NE):
                p.op("pool", lambda e, ee=ee, t=t, xb_=xb_: e.indirect_dma_start(
                    out=xe_d[ee][:, :], out_offset=bass.IndirectOffsetOnAxis(ap=pos_i[:, ee, t:t + 1], axis=0),
                    in_=xb_[:], in_offset=None, bounds_check=bcreg(e), oob_is_err=False),
                     reads=[xb_, pos_i], writes=[], dsem=sX[2 + k % 8])
                k += 1
        p.pop()
        if stop_after == "X":
            return None

        p.push()
        sD = p.dsems(6, "sw") + p.dsems(2) + p.dsems(4, "sw")
        wgb = [p.sb([128, 8, D], BF16, "wg%d" % i) for i in range(2)]
        wub = [p.sb([128, 8, D], BF16, "wu%d" % i) for i in range(2)]
        wdb = [p.sb([128, 8, D], BF16, "wd%d" % i) for i in range(2)]
        xeb = [p.sb([128, KT, RW], BF16, "xeb%d" % i) for i in range(2)]
        xeT = p.sb([128, 8, CAP], BF16, "xeT")
        hidT = p.sb([128, 8, CAP], BF16, "hidT")
        sgb = [p.sb([128, NS], F32, "sgb%d" % i) for i in range(2)]
        yob = [p.sb([128, D], F32, "yob%d" % i) for i in range(3)]
        gsc = [p.sb([128, 1], F32, "gsc%d" % i) for i in range(3)]
        idxi = [p.sb([128, 1], I32, "idxi%d" % i) for i in range(3)]
        idxf = p.sb([128, 1], F32, "idxf")
        hres = Res("hacc")
        turn = p.sb([128, 8], F32, "turn")
        yi = 0
        pXe = p.ps([128, 8, 128], BF16, "pXe")
        pGt = [p.ps([128, 512], F32, "pGt%d" % i) for i in range(2)]
        pUp = [p.ps([128, 512], F32, "pUp%d" % i) for i in range(2)]
        pDb = [p.ps([128, 512], F32, "pDb%d" % i) for i in range(3)]
        dbi = 0

        def d_loads(ee):
            b = ee % 2
            p.dma("pool", wgb[b][:], W["w_gate"][l, ee].rearrange("(k p) n -> p k n", p=128), sD[0 + b], writes=[wgb[b]])
            p.dma("pool", wub[b][:], W["w_up"][l, ee].rearrange("(k p) n -> p k n", p=128), sD[2 + b], writes=[wub[b]])
            p.dma("pool", wdb[b][:], W["w_down"][l, ee].rearrange("(k p) n -> p k n", p=128), sD[4 + b], writes=[wdb[b]])
            p.dma("sp", xeb[b][:], xe_d[ee][:, :].rearrange("(j p) d -> p j d", p=128), sD[6 + b], writes=[xeb[b]])

        d_loads(0)
        gi = 0
        for ee in range(NE):
            if ee + 1 < NE:
                d_loads(ee + 1)
            b = ee % 2
            wg_, wu_, wd_, xe_ = wgb[b], wub[b], wdb[b], xeb[b]
            for j in range(KT):
                p.op("pe", [lambda e, k=k, j=j, xe_=xe_: e.transpose(
                    out=pXe[:, k, :], in_=xe_[:, j, k * 128:(k + 1) * 128], identity=identb[:]) for k in range(8)],
                     [xe_, identb], [pXe])
                p.cp("act" if j % 2 == 0 else "dve", xeT[:, :, j * 128:(j + 1) * 128], pXe[:], [pXe], [xeT])
            for ft in range(8):
                fs = slice(ft * 128, (ft + 1) * 128)
                for sh in range(NSH):
                    ssl = slice(sh * NS, (sh + 1) * NS)
                    pg_, pu_ = pGt[gi % 2], pUp[gi % 2]
                    sg_ = sgb[gi % 2]
                    gi += 1
                    p.op("pe", [lambda e, k=k, fs=fs, ssl=ssl, pg_=pg_, wg_=wg_: e.matmul(
                        pg_[:, 0:NS], lhsT=wg_[:, k, fs], rhs=xeT[:, k, ssl], start=(k == 0), stop=(k == 7))
                                for k in range(8)], [wg_, xeT], [pg_])
                    p.op("pe", [lambda e, k=k, fs=fs, ssl=ssl, pu_=pu_, wu_=wu_: e.matmul(
                        pu_[:, 0:NS], lhsT=wu_[:, k, fs], rhs=xeT[:, k, ssl], start=(k == 0), stop=(k == 7))
                                for k in range(8)], [wu_, xeT], [pu_])
                    p.act(sg_[:], pg_[:, 0:NS], AF.Silu, [pg_], [sg_])
                    p.tt("dve", hidT[:, ft, ssl], sg_[:], pu_[:, 0:NS], ALU.mult, [sg_, pu_], [hidT])
            for j in range(KT):
                js = slice(j * 128, (j + 1) * 128)
                pdh = []
                for n2 in range(2):
                    pd_ = pDb[dbi % 3]
                    dbi += 1
                    pdh.append(pd_)
                    p.op("pe", [lambda e, ft=ft, n2=n2, js=js, wd_=wd_, pd_=pd_: e.matmul(
                        pd_[:], lhsT=hidT[:, ft, js], rhs=wd_[:, ft, n2 * 512:(n2 + 1) * 512],
                        start=(ft == 0), stop=(ft == 7)) for ft in range(8)], [hidT, wd_], [pd_])
                yo_ = yob[yi % 3]
                yi += 1
                gs_ = gsc[yi % 3]
                p.tt("dve", gs_[:], xe_[:, j, 1024 + ee:1025 + ee], xe_[:, j, 1040 + ee:1041 + ee], ALU.add, [xe_], [gs_])
                gate_ap = gs_[:]
                idx_ap = xe_[:, j, 1056:1058].bitcast(I32)
                p.act(yo_[:, 0:512], pdh[0][:], AF.Identity, [pdh[0], gs_], [yo_], scale=gate_ap)
                p.ts("dve", yo_[:, 512:1024], pdh[1][:], gate_ap, None, ALU.mult, None, [pdh[1], gs_], [yo_])
                if j == 0:
                    p.op("pool", lambda e: e.memset(turn[:], 0.0), writes=[hres, turn])
                p.op("pool", lambda e, yo_=yo_, idx_ap=idx_ap: e.indirect_dma_start(
                    out=h1_d[:, :], out_offset=bass.IndirectOffsetOnAxis(ap=idx_ap, axis=0),
                    in_=yo_[:], in_offset=None, bounds_check=sreg(e), oob_is_err=True, compute_op=ALU.add),
                     reads=[yo_, xe_, hres], writes=[], dsem=sD[8 + (yi % 4)])
        p.pop()
        if stop_after == "D":
            return None

        p.pop()
        if last:
            p.push()
            sE = p.dsems(6)
            gfb = p.sb([128, D], F32, "gfb")
            p.dma("sp", gfb[:], W["final_norm_g"][0:1, :].partition_broadcast(128), sE[0], writes=[gfb])
            ssf = p.sb([128, 1], F32, "ssf")
            rsf = p.sb([128, 1], F32, "rsf")
            junke = p.sb([128, D], BF16, "junke")
            accb = [p.sb([128, D], F32, "acc%d" % i) for i in range(2)]
            outb = [p.sb([128, D], F32, "outb%d" % i) for i in range(2)]
            for t in range(NT):
                r = slice(t * 128, (t + 1) * 128)
                acc = accb[t % 2]
                p.dma("sp", acc[:], h1_d[r, :], sE[1 + t % 2], reads=[h1_d], writes=[acc])
                p.act(junke[:], acc[:], AF.Square, [acc], [junke, ssf], accum_out=ssf[:])
                p.act(rsf[:], ssf[:], AF.Ln, [ssf, epsc], [rsf], scale=1.0 / D, bias=epsc[:])
                p.act(rsf[:], rsf[:], AF.Exp, [rsf], [rsf], scale=-0.5)
                ob_ = outb[t % 2]
                p.stt(ob_[:], acc[:], rsf[:], gfb[:], ALU.mult, ALU.mult, [acc, rsf, gfb], [ob_])
                p.dma("sp", out_d[r, :], ob_[:], sE[3 + t % 2], reads=[ob_], writes=[out_d])
            p.pop()
        if stop_after == "E":
            return None
        return h_next


    h_cur = x_in
    for l in range(L):
        h_cur = layer(l, h_cur)
        if h_cur is None:
            break

    p.emit()
    p.close()
    return nc


_CACHE = {}


def kernel(x, norm1_g, w_in, ret_norm_g, hgrn_norm_g, w_out, lower_bounds, norm2_g, w_router,
           w_gate, w_up, w_down, final_norm_g):
    x = np.asarray(x, dtype=np.float32)
    B, S, _ = x.shape
    L = int(np.asarray(w_in).shape[0])
    key = (S, L)
    if key not in _CACHE:
        _CACHE[key] = build(S, L)
    nc = _CACHE[key]
    consts = make_consts(S)
    shared = {
        "norm1_g": norm1_g, "w_in": w_in, "ret_norm_g": ret_norm_g, "hgrn_norm_g": hgrn_norm_g,
        "w_out": w_out, "lower_bounds": lower_bounds, "norm2_g": norm2_g, "w_router": w_router,
        "w_gate": w_gate, "w_up": w_up, "w_down": w_down,
    }
    shared = {k: np.ascontiguousarray(np.asarray(v, dtype=np.float32)) for k, v in shared.items()}
    shared["final_norm_g"] = np.ascontiguousarray(np.asarray(final_norm_g, dtype=np.float32).reshape(1, D))
    shared.update(consts)
    in_maps = []
    for b in range(B):
        m = dict(shared)
        m["x"] = np.ascontiguousarray(x[b])
        in_maps.append(m)
    res = run_bass_kernel_spmd(nc, in_maps, core_ids=list(range(B)))
    return np.stack([np.asarray(r["out"], dtype=np.float32) for r in res.results], axis=0)
```
